# Optimizing a Trainium2 kernel written in Bass

```python
import math
import jax, jax.numpy as jnp
from jax import lax
import numpy as np

D_MODEL = 2048
BATCH = 2
SEQ = 16384
DEPTH = 4

CHUNK = 64
N_MIXERS = 4
D_MIX = D_MODEL
GROUP_W = D_MIX // N_MIXERS
MLA_HEADS = 4
QK_NOPE = 128
QK_ROPE = 64
V_HEAD = 128
KV_RANK = 512
ROPE_THETA = 10000.0
Q_BLOCK = 128
CONV_W = 3
CONV_HEADS = 4
CONV_CH = GROUP_W
POOL_WINDOWS = (2, 4, 8, 16)
POOL_GROUPS = 4
POOL_CH = GROUP_W // POOL_GROUPS
SGU_LEN = 128
SGU_GROUPS = 4
SGU_CH = GROUP_W // SGU_GROUPS
D_FF = 5632
N_EXPERTS = 8
TOP_K = 2
EXPERT_BLOCK = 256
N_DENSE = (DEPTH + 1) // 2
N_MOE = DEPTH // 2
ALPHA = (2.0 * DEPTH) ** 0.25
BETA = (8.0 * DEPTH) ** -0.25
LN_EPS = 1e-5
RMS_EPS = 1e-6

Q_DIM = MLA_HEADS * (QK_NOPE + QK_ROPE)
SPLIT_SIZES = (Q_DIM, KV_RANK, QK_ROPE, CONV_CH, CONV_CH, CONV_CH, GROUP_W, GROUP_W, GROUP_W)
IN_COLS = sum(SPLIT_SIZES)

kernel_name = 'hybrid_mla_conv_pool_sgu_moe_deepnorm'

F32 = jnp.float32


def layer_norm(x, g, b):
    xf = x.astype(F32)
    mu = jnp.mean(xf, axis=-1, keepdims=True)
    var = jnp.mean(jnp.square(xf - mu), axis=-1, keepdims=True)
    return ((xf - mu) * lax.rsqrt(var + LN_EPS) * g + b).astype(x.dtype)


def rms_normalise(x):
    xf = x.astype(F32)
    return (xf * lax.rsqrt(jnp.mean(xf * xf, axis=-1, keepdims=True) + RMS_EPS)).astype(x.dtype)


def rope_tables(positions):
    inv_freq = ROPE_THETA ** (-jnp.arange(0, QK_ROPE, 2, dtype=F32) / QK_ROPE)
    ang = positions.astype(F32)[..., None] * inv_freq
    return jnp.cos(ang), jnp.sin(ang)


def apply_rope(x, cos, sin):
    xf = x.astype(F32)
    x1, x2 = jnp.split(xf, 2, axis=-1)
    return jnp.concatenate([x1 * cos - x2 * sin, x1 * sin + x2 * cos], axis=-1).astype(x.dtype)


def mla_attention(q, c_kv, k_rope, kv_norm_g, w_ukv, cos, sin):
    B, S, _ = q.shape
    q = q.reshape(B, S, MLA_HEADS, QK_NOPE + QK_ROPE)
    q_nope = q[..., :QK_NOPE]
    q_rope = apply_rope(q[..., QK_NOPE:], cos[:, :, None, :], sin[:, :, None, :])
    k_rope = apply_rope(k_rope, cos, sin)
    kv = jnp.einsum('bsr,rn->bsn', rms_normalise(c_kv) * kv_norm_g, w_ukv)
    kv = kv.reshape(B, S, MLA_HEADS, QK_NOPE + V_HEAD)
    k_nope, v = kv[..., :QK_NOPE], kv[..., QK_NOPE:]
    scale = 1.0 / math.sqrt(QK_NOPE + QK_ROPE)
    n_blk = S // Q_BLOCK
    k_chunk = jnp.arange(S) // CHUNK

    def query_block(args):
        qn, qr, blk = args
        s = (jnp.einsum('bqhd,bkhd->bhqk', qn, k_nope)
             + jnp.einsum('bqhr,bkr->bhqk', qr, k_rope))
        q_chunk = (blk * Q_BLOCK + jnp.arange(Q_BLOCK)) // CHUNK
        allowed = k_chunk[None, :] <= q_chunk[:, None]
        s = jnp.where(allowed, s.astype(F32) * scale, -jnp.inf)
        p = jax.nn.softmax(s, axis=-1).astype(v.dtype)
        return jnp.einsum('bhqk,bkhd->bqhd', p, v)

    def to_blocks(t):
        return jnp.moveaxis(t.reshape(B, n_blk, Q_BLOCK, *t.shape[2:]), 1, 0)

    o = lax.map(query_block, (to_blocks(q_nope), to_blocks(q_rope), jnp.arange(n_blk)))
    return jnp.moveaxis(o, 0, 1).reshape(B, S, MLA_HEADS * V_HEAD)


def short_conv(b_gate, c_gate, h, conv_w):
    S = h.shape[1]
    g = c_gate * h
    gp = jnp.pad(g, ((0, 0), (CONV_W - 1, 0), (0, 0)))
    conv = conv_w[0] * gp[:, 0:S]
    for j in range(1, CONV_W):
        conv = conv + conv_w[j] * gp[:, j:j + S]
    return b_gate * conv


def pool_mixer(h, pool_w):
    B, S, _ = h.shape
    hf = h.astype(F32)
    cs = jnp.cumsum(hf, axis=1)
    t = jnp.arange(S)
    outs = []
    for g, w in enumerate(POOL_WINDOWS):
        sl = slice(g * POOL_CH, (g + 1) * POOL_CH)
        csg = cs[..., sl]
        lagged = jnp.pad(csg, ((0, 0), (w, 0), (0, 0)))[:, :S]
        count = jnp.minimum(t + 1, w).astype(F32)[None, :, None]
        outs.append((csg - lagged) / count - hf[..., sl])
    p = jnp.stack(outs, axis=2).astype(h.dtype)
    return jnp.einsum('bsgc,gcd->bsgd', p, pool_w).reshape(B, S, GROUP_W)


def spatial_gating(u, v, ln_g, ln_b, sgu_w, sgu_b):
    B, S, _ = u.shape
    u = jax.nn.gelu(u, approximate=False)
    v = layer_norm(jax.nn.gelu(v, approximate=False), ln_g, ln_b)
    n = S // SGU_LEN
    causal = jnp.tril(jnp.ones((SGU_LEN, SGU_LEN), dtype=bool))
    w = jnp.where(causal[None], sgu_w, jnp.zeros_like(sgu_w))
    vb = v.reshape(B, n, SGU_LEN, SGU_GROUPS, SGU_CH)
    mixed = jnp.einsum('gts,bnsgc->bntgc', w, vb) + jnp.transpose(sgu_b)[None, None, :, :, None]
    return u * mixed.reshape(B, S, GROUP_W)


def swiglu(x, wg, wu, wd):
    return jnp.matmul(jax.nn.silu(jnp.matmul(x, wg)) * jnp.matmul(x, wu), wd)


def moe_ffn(x, router_w, wg, wu, wd):
    B, S, D = x.shape
    T = B * S
    A = T * TOP_K
    xf = x.reshape(T, D)
    logits = jnp.matmul(xf, router_w).astype(F32)
    top_logit, top_e = lax.top_k(logits, TOP_K)
    gates = jax.nn.softmax(top_logit, axis=-1)
    flat_e = top_e.reshape(A)
    order = jnp.argsort(flat_e)
    sorted_e = flat_e[order]
    tok = (order // TOP_K).astype(jnp.int32)
    gate_sorted = gates.reshape(A)[order]
    counts = jnp.bincount(flat_e, length=N_EXPERTS)
    padded = (counts + EXPERT_BLOCK - 1) // EXPERT_BLOCK * EXPERT_BLOCK
    starts = jnp.cumsum(counts) - counts
    pad_ends = jnp.cumsum(padded)
    pad_starts = pad_ends - padded
    dest = pad_starts[sorted_e] + jnp.arange(A) - starts[sorted_e]
    n_blocks = -(-(A + N_EXPERTS * (EXPERT_BLOCK - 1)) // EXPERT_BLOCK)
    n_pad = n_blocks * EXPERT_BLOCK
    slot_tok = jnp.full((n_pad,), T, dtype=jnp.int32).at[dest].set(tok)
    x_rows = jnp.concatenate([xf, jnp.zeros((1, D), xf.dtype)], axis=0)[slot_tok]
    x_rows = x_rows.reshape(n_blocks, EXPERT_BLOCK, D)
    block_e = jnp.minimum(
        jnp.searchsorted(pad_ends, jnp.arange(n_blocks) * EXPERT_BLOCK, side='right'), N_EXPERTS - 1)

    def expert_block(args):
        xb, e = args
        return swiglu(xb, wg[e], wu[e], wd[e])

    y_rows = lax.map(expert_block, (x_rows, block_e)).reshape(n_pad, D)
    contrib = y_rows[dest] * gate_sorted[:, None].astype(x.dtype)
    return jax.ops.segment_sum(contrib, tok, num_segments=T).reshape(B, S, D)


def setup_inputs(seed: int = 0) -> dict:
    key = jax.random.key(seed)
    ks = jax.random.split(key, 26)
    L = DEPTH

    def nrm(k, shape, scale):
        return jax.random.normal(k, shape, F32) * scale

    x = nrm(ks[0], (BATCH, SEQ, D_MODEL), 1.0)
    positions = (jax.random.randint(ks[1], (BATCH, 1), 0, 4096, dtype=jnp.int32)
                 + jnp.arange(SEQ, dtype=jnp.int32)[None, :])
    return {
        'x': x,
        'positions': positions,
        'w_in': nrm(ks[2], (L, D_MODEL, IN_COLS), D_MODEL ** -0.5),
        'kv_norm_g': 1.0 + nrm(ks[3], (L, KV_RANK), 0.02),
        'w_ukv': nrm(ks[4], (L, KV_RANK, MLA_HEADS * (QK_NOPE + V_HEAD)), KV_RANK ** -0.5),
        'conv_w': nrm(ks[5], (L, CONV_W, CONV_CH), CONV_W ** -0.5),
        'pool_w': nrm(ks[6], (L, POOL_GROUPS, POOL_CH, POOL_CH), POOL_CH ** -0.5),
        'sgu_ln_g': 1.0 + nrm(ks[7], (L, GROUP_W), 0.02),
        'sgu_ln_b': nrm(ks[8], (L, GROUP_W), 0.02),
        'sgu_w': nrm(ks[9], (L, SGU_GROUPS, SGU_LEN, SGU_LEN), SGU_LEN ** -0.5),
        'sgu_b': 1.0 + nrm(ks[10], (L, SGU_GROUPS, SGU_LEN), 0.02),
        'mix_gain': 1.0 + nrm(ks[11], (L, D_MIX), 0.02),
        'w_o': nrm(ks[12], (L, D_MIX, D_MODEL), BETA * D_MIX ** -0.5),
        'ln1_g': 1.0 + nrm(ks[13], (L, D_MODEL), 0.02),
        'ln1_b': nrm(ks[14], (L, D_MODEL), 0.02),
        'ffn_wg': nrm(ks[15], (N_DENSE, D_MODEL, D_FF), D_MODEL ** -0.5),
        'ffn_wu': nrm(ks[16], (N_DENSE, D_MODEL, D_FF), D_MODEL ** -0.5),
        'ffn_wd': nrm(ks[17], (N_DENSE, D_FF, D_MODEL), BETA * D_FF ** -0.5),
        'router_w': nrm(ks[18], (N_MOE, D_MODEL, N_EXPERTS), D_MODEL ** -0.5),
        'exp_wg': nrm(ks[19], (N_MOE, N_EXPERTS, D_MODEL, D_FF), D_MODEL ** -0.5),
        'exp_wu': nrm(ks[20], (N_MOE, N_EXPERTS, D_MODEL, D_FF), D_MODEL ** -0.5),
        'exp_wd': nrm(ks[21], (N_MOE, N_EXPERTS, D_FF, D_MODEL), BETA * D_FF ** -0.5),
        'ln2_g': 1.0 + nrm(ks[22], (L, D_MODEL), 0.02),
        'ln2_b': nrm(ks[23], (L, D_MODEL), 0.02),
    }


def reference(x, positions, w_in, kv_norm_g, w_ukv, conv_w, pool_w, sgu_ln_g, sgu_ln_b, sgu_w, sgu_b,
              mix_gain, w_o, ln1_g, ln1_b, ffn_wg, ffn_wu, ffn_wd, router_w, exp_wg, exp_wu, exp_wd,
              ln2_g, ln2_b):
    B, S, _ = x.shape
    cos, sin = rope_tables(positions)
    offsets = [sum(SPLIT_SIZES[:i + 1]) for i in range(len(SPLIT_SIZES) - 1)]
    for l in range(DEPTH):
        proj = jnp.einsum('bsd,dn->bsn', x, w_in[l])
        q, c_kv, k_rope, cb, cc, ch, ph, gu, gv = jnp.split(proj, offsets, axis=-1)
        y_mla = mla_attention(q, c_kv, k_rope, kv_norm_g[l], w_ukv[l], cos, sin)
        y_conv = short_conv(cb, cc, ch, conv_w[l])
        y_pool = pool_mixer(ph, pool_w[l])
        y_sgu = spatial_gating(gu, gv, sgu_ln_g[l], sgu_ln_b[l], sgu_w[l], sgu_b[l])
        groups = jnp.stack([y_mla, y_conv, y_pool, y_sgu], axis=2)
        merged = (rms_normalise(groups) * mix_gain[l].reshape(N_MIXERS, GROUP_W)).reshape(B, S, D_MIX)
        mix = jnp.matmul(merged, w_o[l])
        x = layer_norm(ALPHA * x + mix, ln1_g[l], ln1_b[l])
        if l % 2 == 0:
            ffn = swiglu(x, ffn_wg[l // 2], ffn_wu[l // 2], ffn_wd[l // 2])
        else:
            ffn = moe_ffn(x, router_w[l // 2], exp_wg[l // 2], exp_wu[l // 2], exp_wd[l // 2])
        x = layer_norm(ALPHA * x + ffn, ln2_g[l], ln2_b[l])
    return x
```

```python
import contextlib
import numpy as np
import concourse.bass as bass
import concourse.mybir as mybir
from concourse.bass_utils import run_bass_kernel_spmd

F32 = mybir.dt.float32
BF16 = mybir.dt.bfloat16
I32 = mybir.dt.int32
U32 = mybir.dt.uint32
AF = mybir.ActivationFunctionType
ALU = mybir.AluOpType
AX = mybir.AxisListType


class Buf:
    __slots__ = ("name", "last_w", "readers", "dsem", "psum")

    def __init__(self, name):
        self.name = name
        self.last_w = None
        self.readers = []
        self.dsem = None
        self.psum = False


class Sem:
    def __init__(self, handle, step):
        self.h = handle
        self.step = step
        self.count = 0


class Eng:
    def __init__(self, name, handle, sem):
        self.name = name
        self.h = handle
        self.sem = sem
        self.waited = {}
        self.ops = []


class _Rec:
    def __init__(self):
        self.call = None

    def __getattr__(self, name):
        def f(*a, **k):
            self.call = (name, a, k)
            return self
        return f

    def then_inc(self, *a, **k):
        return self


class Ctx:
    def __init__(self, nc, stack):
        self.nc = nc
        self.stack = stack
        self.engs = {}
        for name, h in (("pe", nc.tensor), ("act", nc.scalar), ("dve", nc.vector),
                        ("pool", nc.gpsimd), ("sp", nc.sync)):
            s = Sem(stack.enter_context(nc.semaphore("e_" + name)), 1)
            self.engs[name] = Eng(name, h, s)
        self.top = stack
        self.dsems = []
        self.free_dsems = {}
        self.nbuf = 0

    def sbuf(self, name, shape, dtype):
        self.nbuf += 1
        name = "s%d_%s" % (self.nbuf, name)
        t = self.stack.enter_context(self.nc.sbuf_tensor(name, list(shape), dtype))
        b = Buf(name)
        return t, b

    def psum(self, name, shape, dtype=F32):
        self.nbuf += 1
        name = "p%d_%s" % (self.nbuf, name)
        t = self.stack.enter_context(self.nc.psum_tensor(name, list(shape), dtype))
        b = Buf(name)
        b.psum = True
        return t, b

    def dsem_for(self, buf, kind):
        if buf.dsem is None:
            buf.dsem = {}
        if kind not in buf.dsem:
            fl = self.free_dsems.setdefault(kind, [])
            if fl:
                s = fl.pop()
            else:
                s = Sem(self.top.enter_context(self.nc.semaphore("d%s%d" % (kind, len(self.dsems)))), 16)
                self.dsems.append(s)
            buf.dsem[kind] = s
        return buf.dsem[kind]

    def _deps(self, reads, writes):
        deps = []
        for b in reads:
            if b.last_w is not None:
                deps.append(b.last_w)
            if b.psum:
                deps.extend(b.readers)
        for b in writes:
            if b.last_w is not None:
                deps.append(b.last_w)
            deps.extend(b.readers)
        return deps

    def _emit_waits(self, eng, deps, skip_sem=None):
        need = {}
        for (s, v) in deps:
            if s is skip_sem:
                continue
            if eng.waited.get(id(s), 0) >= v:
                continue
            if need.get(id(s), (None, 0))[1] < v:
                need[id(s)] = (s, v)
        for (s, v) in need.values():
            eng.waited[id(s)] = v
            eng.ops.append(("wait", s.h, v))

    def op(self, ename, fn, reads=(), writes=(), pe_chain=False):
        eng = self.engs[ename]
        deps = self._deps(reads, writes)
        self._emit_waits(eng, deps, skip_sem=eng.sem if pe_chain else None)
        eng.sem.count += 1
        tok = (eng.sem, eng.sem.count)
        rec = _Rec()
        fn(rec)
        nm, a, k = rec.call
        eng.ops.append(("op", (lambda e, nm=nm, a=a, k=k: getattr(e, nm)(*a, **k)), eng.sem.h, 1))
        for b in writes:
            b.last_w = tok
            b.readers = []
        for b in reads:
            b.readers.append(tok)
        return tok

    def dma(self, ename, out, in_, reads=(), writes=(), sembuf=None, **kw):
        eng = self.engs[ename]
        deps = self._deps(reads, writes)
        self._emit_waits(eng, deps)
        s = self.dsem_for(sembuf, 'sw' if ename == 'pool' else 'hw')
        s.count += 1
        tok = (s, s.count * 16)
        eng.ops.append(("op", (lambda e, o=out, i=in_, k=kw: e.dma_start(out=o, in_=i, **dict(dict(allow_slow_non_contiguous=True), **k))), s.h, 16))
        for b in writes:
            b.last_w = tok
            b.readers = []
        for b in reads:
            b.readers.append(tok)
        return tok

    def barrier(self, bufs):
        for eng in self.engs.values():
            deps = []
            for b in bufs:
                if b.last_w is not None:
                    deps.append(b.last_w)
                deps.extend(b.readers)
            for s in self.dsems:
                if s.count > 0:
                    deps.append((s, s.count * 16))
            for e2 in self.engs.values():
                if e2.sem.count > 0:
                    deps.append((e2.sem, e2.sem.count))
            self._emit_waits(eng, deps)
        for b in bufs:
            if b.dsem is not None:
                for kind, sm in b.dsem.items():
                    self.free_dsems.setdefault(kind, []).append(sm)
                b.dsem = None

    def finish(self, out_bufs):
        sp = self.engs["sp"]
        deps = []
        for b in out_bufs:
            if b.last_w is not None:
                deps.append(b.last_w)
        for e in self.engs.values():
            if e.sem.count > 0:
                deps.append((e.sem, e.sem.count))
        for s in self.dsems:
            if s.count > 0:
                deps.append((s, s.count * 16))
        self._emit_waits(sp, deps)
        self.flush()

    def flush(self):
        nc = self.nc
        pend = {k: e.ops for k, e in self.engs.items()}
        for e in self.engs.values():
            e.ops = []
        with nc.Block() as block:
            def replay(e, ops):
                for o in ops:
                    if o[0] == "wait":
                        e.wait_ge(o[1], o[2])
                    else:
                        ins = o[1](e)
                        ins.then_inc(o[2], o[3])

            @block.tensor
            def _(e):
                replay(e, pend["pe"])

            @block.scalar
            def _(e):
                replay(e, pend["act"])

            @block.vector
            def _(e):
                replay(e, pend["dve"])

            @block.gpsimd
            def _(e):
                replay(e, pend["pool"])

            @block.sync
            def _(e):
                replay(e, pend["sp"])

import contextlib, math
import numpy as np

D = 2048
INC = 4416
XH = 16
TT = 512
EPS_RMS = 1e-6
EPS_LN = 1e-5
TWO_PI = 2.0 * math.pi
C1 = 6.28125
_rem = TWO_PI - C1
C2 = float(np.frombuffer((np.array([_rem], np.float32).view(np.uint32) & np.uint32(0xFFFFF000)).tobytes(), np.float32)[0])
C3 = float(np.float32(TWO_PI - C1 - C2))
MAGIC = 12582912.0
PI_CL = 3.1415925


class Ring:
    def __init__(self, c, name, shape, dtype, n, psum=False):
        self.items = []
        for i in range(n):
            self.items.append((c.psum if psum else c.sbuf)("%s%d" % (name, i), shape, dtype))
        self.i = 0

    def next(self):
        r = self.items[self.i % len(self.items)]
        self.i += 1
        return r


def consts(c):
    K = {}
    idf, idfb = c.sbuf("idf", [128, 128], F32)
    c.op("pool", lambda e: e.memset(idf[:], 1.0), writes=[idfb])
    c.op("pool", lambda e: e.affine_select(out=idf[:], in_=idf[:], pattern=[[-1, 128]], compare_op=ALU.is_equal,
                                           fill=0.0, base=0, channel_multiplier=1), reads=[idfb], writes=[idfb])
    idh, idhb = c.sbuf("idh", [128, 128], BF16)
    c.op("dve", lambda e: e.tensor_copy(out=idh[:], in_=idf[:]), reads=[idfb], writes=[idhb])
    ones, onesb = c.sbuf("ones", [128, 128], BF16)
    c.op("pool", lambda e: e.memset(ones[:], 1.0), writes=[onesb])
    ce, ceb = c.sbuf("ceps", [128, 4], F32)
    c.op("pool", lambda e: e.memset(ce[:, 0:1], EPS_RMS), writes=[ceb])
    c.op("pool", lambda e: e.memset(ce[:, 1:2], EPS_LN), writes=[ceb])
    c.op("pool", lambda e: e.memset(ce[:, 2:3], 0.0), writes=[ceb])
    K.update(idf=(idf, idfb), idh=(idh, idhb), ones=(ones, onesb), ce=(ce, ceb))
    return K


def load_cols(c, K, name, src2d, nrow):
    idf, idfb = K["idf"]
    t, tb = c.sbuf(name + "_r", [nrow, 128], F32)
    c.dma("sp", t[:], src2d, writes=[tb], sembuf=tb)
    p, pb = K["pcol"]
    c.op("pe", lambda e: e.transpose(p[:, 0:nrow], t[:], idf[0:nrow, 0:nrow]), reads=[tb, idfb], writes=[pb])
    o, ob = c.sbuf(name, [128, nrow], F32)
    c.op("dve", lambda e: e.tensor_copy(out=o[:], in_=p[:, 0:nrow]), reads=[pb], writes=[ob])
    return o, ob

def rstd_from(c, eng_out, out_ap, in_ap, scale, eps_ap, reads, wbuf):
    c.op("act", lambda e: e.activation(out=out_ap, in_=in_ap, func=AF.Ln, bias=eps_ap, scale=scale),
         reads=reads, writes=[wbuf])
    c.op("act", lambda e: e.activation(out=out_ap, in_=out_ap, func=AF.Exp, scale=-0.5),
         reads=[wbuf], writes=[wbuf])


def phase_a(c, K, NT, T):
    nc = c.nc
    NTOK = NT * TT
    ones, onesb = K["ones"]
    ce, ceb = K["ce"]
    idf, idfb = K["idf"]
    idh, idhb = K["idh"]
    xT = T["xT"]
    xTv = xT.rearrange("(c p) t -> p c t", p=128)
    w_in = T["w_in"].rearrange("(c p) n -> p c n", p=128)
    dq = Buf("dram_q"); dk = Buf("dram_k"); dv = Buf("dram_v"); dm = Buf("dram_m")

    import os
    PARTS = os.environ.get('PA_PARTS', '123')
    STOP = int(os.environ.get('PA_STOP', '99'))
    S1 = int(os.environ.get('PA_S1', '99'))
    SKIP = os.environ.get('PA_SKIP', '').split(',')
    with contextlib.ExitStack() as st:
      if '1' in PARTS:
          c.stack, old = st, c.stack
          wq, wqb = c.sbuf("wq", [128, 16, 768], BF16)
          wc, wcb = c.sbuf("wc", [128, 16, 576], BF16)
          wqr, wqrb = c.sbuf("wqr", [128, 16, 4, 64], BF16)
          wkr, wkrb = c.sbuf("wkr", [128, 16, 64], BF16)
          wuf, wufb = c.sbuf("wuf", [128, 4, 1024], F32)
          wu, wub = c.sbuf("wu", [128, 4, 1024], BF16)
          invf, invfb = c.sbuf("invf", [64, 1], F32)
          for k0 in range(0, 16, 4):
              c.dma("pool", wq[:, k0:k0 + 4, :], w_in[:, k0:k0 + 4, 0:768], writes=[wqb], sembuf=wqb)
              c.dma("pool", wc[:, k0:k0 + 4, :], w_in[:, k0:k0 + 4, 768:1344], writes=[wcb], sembuf=wcb)
          c.dma("sp", wuf[:], T["w_ukv"].rearrange("(c p) n -> p c n", p=128), writes=[wufb], sembuf=wufb)
          K["pcol"] = c.psum("pcol", [128, 16], F32)
          kg, kgb = load_cols(c, K, "kg", T["kvg"].rearrange("(c p) -> c p", p=128), 4)
          c.dma("sp", invf[:], T["invf"].rearrange("(p o) -> p o", o=1), writes=[invfb], sembuf=invfb)
          for k in range(4):
              c.op("dve", lambda e, k=k: e.tensor_scalar(out=wu[:, k, :], in0=wuf[:, k, :], scalar1=kg[:, k:k + 1],
                                                         scalar2=None, op0=ALU.mult), reads=[wufb, kgb], writes=[wub])
          wq4 = wq[:].rearrange("p c (h d) -> p c h d", h=4)
          for k in range(16):
              c.op("dve", lambda e, k=k: e.tensor_scalar(out=wqr[:, k, :, 0:32], in0=wq4[:, k, :, 160:192], scalar1=-1.0,
                                                         scalar2=None, op0=ALU.mult), reads=[wqb], writes=[wqrb])
              c.op("pool", lambda e, k=k: e.tensor_copy(out=wqr[:, k, :, 32:64], in_=wq4[:, k, :, 128:160]),
                   reads=[wqb], writes=[wqrb])
          c.op("dve", lambda e: e.tensor_scalar(out=wkr[:, :, 0:32], in0=wc[:, :, 544:576], scalar1=-1.0, scalar2=None,
                                                op0=ALU.mult), reads=[wcb], writes=[wkrb])
          c.op("pool", lambda e: e.tensor_copy(out=wkr[:, :, 32:64], in_=wc[:, :, 512:544]), reads=[wcb], writes=[wkrb])

          xr = Ring(c, "xt", [128, 16, TT], BF16, 2)
          posi, posib = c.sbuf("posi", [64, TT], I32)
          ang, angb = c.sbuf("ang", [64, TT], F32)
          rn, rnb = c.sbuf("rn", [64, TT], F32)
          rr, rrb = c.sbuf("rr", [64, TT], F32)
          sn, snb = c.sbuf("sin", [64, TT], F32)
          cs, csb = c.sbuf("cos", [64, TT], F32)
          t1r = Ring(c, "t1", [64, TT], F32, 2)
          t2r = Ring(c, "t2", [64, TT], F32, 2)
          obr = Ring(c, "ob", [128, TT], BF16, 4)
          ovr = Ring(c, "ov", [128, 512], BF16, 2)
          sq, sqb = c.sbuf("sq", [128, 4, TT], BF16)
          ck, ckb = c.sbuf("ck", [128, 4, TT], BF16)
          rsF, rsFb = c.sbuf("rsF", [128, TT], F32)
          rsT, rsTb = c.sbuf("rsT", [128, 4], F32)
          pr = Ring(c, "pA", [128, TT], F32, 4, psum=True)
          pr2 = Ring(c, "pB", [128, TT], F32, 1, psum=True)
          pss, pssb = c.psum("pss", [128, TT], F32)
          pst, pstb = c.psum("pst", [128, 4], F32)

          for i in range(NT):
              col0 = XH + i * TT
              xt, xtb = xr.next()
              c.dma("sp", xt[:], xTv[:, :, col0:col0 + TT], writes=[xtb], sembuf=xtb)
              c.dma("sp", posi[:], T["pos"][i * TT:(i + 1) * TT].partition_broadcast(64), writes=[posib], sembuf=posib)
              c.op("dve", lambda e: e.tensor_copy(out=ang[:], in_=posi[:]), reads=[posib], writes=[angb])
              c.op("dve", lambda e: e.tensor_scalar(out=ang[:], in0=ang[:], scalar1=invf[:, 0:1], scalar2=None, op0=ALU.mult),
                   reads=[angb, invfb], writes=[angb])
              c.op("dve", lambda e: e.tensor_scalar(out=rn[:], in0=ang[:], scalar1=1.0 / TWO_PI, scalar2=MAGIC,
                                                    op0=ALU.mult, op1=ALU.add), reads=[angb], writes=[rnb])
              c.op("dve", lambda e: e.tensor_scalar(out=rn[:], in0=rn[:], scalar1=-MAGIC, scalar2=None, op0=ALU.add),
                   reads=[rnb], writes=[rnb])
              c.op("dve", lambda e: e.scalar_tensor_tensor(out=rr[:], in0=rn[:], scalar=-C1, in1=ang[:], op0=ALU.mult, op1=ALU.add),
                   reads=[rnb, angb], writes=[rrb])
              c.op("dve", lambda e: e.scalar_tensor_tensor(out=rr[:], in0=rn[:], scalar=-C2, in1=rr[:], op0=ALU.mult, op1=ALU.add),
                   reads=[rnb, rrb], writes=[rrb])
              c.op("dve", lambda e: e.scalar_tensor_tensor(out=rr[:], in0=rn[:], scalar=-C3, in1=rr[:], op0=ALU.mult, op1=ALU.add),
                   reads=[rnb, rrb], writes=[rrb])
              c.op("dve", lambda e: e.tensor_scalar(out=ang[:], in0=rr[:], scalar1=math.pi / 2, scalar2=-TWO_PI,
                                                    op0=ALU.is_gt, op1=ALU.mult), reads=[rrb], writes=[angb])
              c.op("dve", lambda e: e.scalar_tensor_tensor(out=ang[:], in0=rr[:], scalar=math.pi / 2, in1=ang[:],
                                                           op0=ALU.add, op1=ALU.add), reads=[rrb, angb], writes=[angb])
              c.op("dve", lambda e: e.tensor_scalar(out=rr[:], in0=rr[:], scalar1=PI_CL, scalar2=-PI_CL, op0=ALU.min, op1=ALU.max),
                   reads=[rrb], writes=[rrb])
              c.op("dve", lambda e: e.tensor_scalar(out=ang[:], in0=ang[:], scalar1=PI_CL, scalar2=-PI_CL, op0=ALU.min, op1=ALU.max),
                   reads=[angb], writes=[angb])
              c.op("act", lambda e: e.activation(out=sn[:], in_=rr[:], func=AF.Sin), reads=[rrb], writes=[snb])
              c.op("act", lambda e: e.activation(out=cs[:], in_=ang[:], func=AF.Sin), reads=[angb], writes=[csb])

              def rope_block(wa, wrot, dst_ap, dbuf):
                  pa, pab = pr.next()
                  pb, pbb = pr.next()
                  for k in range(16):
                      c.op("pe", lambda e, k=k: e.matmul(pa[0:64, :], wa(k), xt[:, k, :], start=(k == 0), stop=(k == 15)),
                           reads=[xtb, wqb, wcb], writes=[pab], pe_chain=(k > 0))
                  for k in range(16):
                      c.op("pe", lambda e, k=k: e.matmul(pb[0:64, :], wrot(k), xt[:, k, :], start=(k == 0), stop=(k == 15)),
                           reads=[xtb, wqrb, wkrb], writes=[pbb], pe_chain=(k > 0))
                  t1, t1b = t1r.next()
                  t2, t2b = t2r.next()
                  c.op("dve", lambda e: e.tensor_tensor(out=t1[:], in0=pa[0:64, :], in1=cs[:], op=ALU.mult), reads=[pab, csb], writes=[t1b])
                  c.op("dve", lambda e: e.tensor_tensor(out=t2[:], in0=pb[0:64, :], in1=sn[:], op=ALU.mult), reads=[pbb, snb], writes=[t2b])
                  ob, obb = obr.next()
                  c.op("dve", lambda e: e.tensor_tensor(out=ob[0:64, :], in0=t1[:], in1=t2[:], op=ALU.add), reads=[t1b, t2b], writes=[obb])
                  c.dma("sp", dst_ap, ob[0:64, :], reads=[obb], writes=[dbuf], sembuf=obb)

              for h in range(4 if S1 >= 3 else 0):
                  pa, pab = pr.next()
                  for k in range(16):
                      c.op("pe", lambda e, k=k, h=h: e.matmul(pa[:], wq[:, k, 192 * h:192 * h + 128], xt[:, k, :], start=(k == 0), stop=(k == 15)),
                           reads=[xtb, wqb], writes=[pab], pe_chain=(k > 0))
                  ob, obb = obr.next()
                  c.op("act", lambda e: e.activation(out=ob[:], in_=pa[:], func=AF.Copy), reads=[pab], writes=[obb])
                  c.dma("sp", T["QT"][h, 0:128, i * TT:(i + 1) * TT], ob[:], reads=[obb], writes=[dq], sembuf=obb)
                  rope_block(lambda k, h=h: wq[:, k, 192 * h + 128:192 * h + 192], lambda k, h=h: wqr[:, k, h, :],
                             T["QT"][h, 128:192, i * TT:(i + 1) * TT], dq)
              S1 >= 4 and 'kr' not in SKIP and rope_block(lambda k: wc[:, k, 512:576], lambda k: wkr[:, k, :], T["KT"][512:576, i * TT:(i + 1) * TT], dk)
              for ch in range(4 if S1 >= 4 and 'ckv' not in SKIP else 0):
                  pa, pab = pr.next()
                  for k in range(16):
                      c.op("pe", lambda e, k=k, ch=ch: e.matmul(pa[:], wc[:, k, 128 * ch:128 * ch + 128], xt[:, k, :], start=(k == 0), stop=(k == 15)),
                           reads=[xtb, wcb], writes=[pab], pe_chain=(k > 0))
                  c.op("act", lambda e, ch=ch: e.activation(out=sq[:, ch, :], in_=pa[:], func=AF.Square), reads=[pab], writes=[sqb])
                  c.op("act", lambda e, ch=ch: e.activation(out=ck[:, ch, :], in_=pa[:], func=AF.Copy), reads=[pab], writes=[ckb])
              for ch in range(4 if S1 >= 5 else 0):
                  c.op("pe", lambda e, ch=ch: e.matmul(pss[:], ones[:], sq[:, ch, :], start=(ch == 0), stop=(ch == 3)),
                       reads=[sqb, onesb], writes=[pssb], pe_chain=(ch > 0))
              for sub in range(4 if S1 >= 6 else 0):
                  for ch in range(4):
                      c.op("pe", lambda e, ch=ch, sub=sub: e.matmul(pst[:, sub:sub + 1], sq[:, ch, sub * 128:(sub + 1) * 128], ones[:, 0:1],
                                                                   start=(ch == 0), stop=(ch == 3)),
                           reads=[sqb, onesb], writes=[pstb], pe_chain=(ch > 0 or sub > 0))
              S1 >= 5 and rstd_from(c, "act", rsF[:], pss[:], 1.0 / 512, ce[:, 0:1], [pssb, ceb], rsFb)
              S1 >= 6 and rstd_from(c, "act", rsT[:], pst[:], 1.0 / 512, ce[:, 0:1], [pstb, ceb], rsTb)
              for h in range(4 if S1 >= 7 else 0):
                  pa, pab = pr.next()
                  for k in range(4):
                      c.op("pe", lambda e, k=k, h=h: e.matmul(pa[:], wu[:, k, 256 * h:256 * h + 128], ck[:, k, :], start=(k == 0), stop=(k == 3)),
                           reads=[ckb, wub], writes=[pab], pe_chain=(k > 0))
                  ob, obb = obr.next()
                  c.op("dve", lambda e: e.tensor_tensor(out=ob[:], in0=pa[:], in1=rsF[:], op=ALU.mult), reads=[pab, rsFb], writes=[obb])
                  c.dma("sp", T["KT"][128 * h:128 * h + 128, i * TT:(i + 1) * TT], ob[:], reads=[obb], writes=[dk], sembuf=obb)
              wuv = wu[:].rearrange("p c (h d) -> p c h d", h=4)
              for sub in range(4 if S1 >= 8 else 0):
                  pa, pab = pr2.next()
                  pav = pa[:].rearrange("p (h d) -> p h d", h=4)
                  for k in range(4):
                      c.op("pe", lambda e, k=k, sub=sub: e.matmul(pav, ck[:, k, sub * 128:(sub + 1) * 128], wuv[:, k, :, 128:256], start=(k == 0), stop=(k == 3)),
                           reads=[ckb, wub], writes=[pab], pe_chain=(k > 0))
                  ov, ovb = ovr.next()
                  c.op("dve", lambda e, sub=sub: e.tensor_scalar(out=ov[:], in0=pa[:], scalar1=rsT[:, sub:sub + 1], scalar2=None, op0=ALU.mult),
                       reads=[pab, rsTb], writes=[ovb])
                  t0 = i * TT + sub * 128
                  c.dma("sp", T["V"][t0:t0 + 128, :], ov[:], reads=[ovb], writes=[dv], sembuf=ovb)
          c.barrier([b for (_, b) in xr.items + obr.items + ovr.items + pr.items + pr2.items] + [wqb, wcb, wqrb, wkrb, wub, wufb, sqb, ckb, rsFb, rsTb, pssb, pstb, snb, csb, angb, rrb, rnb, posib, kgb, invfb] + [b for (_, b) in t1r.items + t2r.items])
          c.flush()
          c.stack = old

    with contextlib.ExitStack() as st:
      if '2' in PARTS:
          c.stack, old = st, c.stack
          w3, w3b = c.sbuf("w3", [128, 16, 1536], BF16)
          for k0 in range(0, 16, 2):
              c.dma("pool", w3[:, k0:k0 + 2, :], w_in[:, k0:k0 + 2, 1344:2880], writes=[w3b], sembuf=w3b)
          cw, cwb = c.sbuf("cw", [128, 3, 4], F32)
          c.dma("sp", cw[:], T["conv_w"].rearrange("j (c p) -> p j c", p=128), writes=[cwb], sembuf=cwb)
          gn, gnb = c.sbuf("gain", [128, 16], F32)
          c.dma("sp", gn[:], T["gain"].rearrange("(c p) -> p c", p=128), writes=[gnb], sembuf=gnb)
          xr = Ring(c, "xt", [128, 16, TT], BF16, 2)
          xh, xhb = c.sbuf("xh", [128, 16, XH], BF16)
          gbuf, gbb = c.sbuf("gbuf", [128, 4, 2 + TT], F32)
          hs, hsb = c.sbuf("hs", [128, TT], F32)
          acc, accb = c.sbuf("acc", [128, TT], F32)
          yc, ycb = c.sbuf("yc", [128, 4, TT], F32)
          sq, sqb = c.sbuf("sq", [128, 4, TT], BF16)
          rsF, rsFb = c.sbuf("rsF", [128, TT], F32)
          obr = Ring(c, "ob", [128, TT], BF16, 4)
          pr = Ring(c, "pA", [128, TT], F32, 6, psum=True)
          pss, pssb = c.psum("pss", [128, TT], F32)
          c.dma("sp", xh[:], xTv[:, :, 0:XH], writes=[xhb], sembuf=xhb)
          for ch in range(4 if STOP >= 1 else 0):
              pc, pcb = pr.next()
              ph, phb = pr.next()
              for k in range(16):
                  c.op("pe", lambda e, k=k, ch=ch: e.matmul(pc[:, 0:XH], w3[:, k, 512 + 128 * ch:512 + 128 * ch + 128], xh[:, k, :], start=(k == 0), stop=(k == 15)),
                       reads=[xhb, w3b], writes=[pcb], pe_chain=(k > 0))
              for k in range(16):
                  c.op("pe", lambda e, k=k, ch=ch: e.matmul(ph[:, 0:XH], w3[:, k, 1024 + 128 * ch:1024 + 128 * ch + 128], xh[:, k, :], start=(k == 0), stop=(k == 15)),
                       reads=[xhb, w3b], writes=[phb], pe_chain=(k > 0))
              c.op("act", lambda e: e.activation(out=hs[:, 0:XH], in_=ph[:, 0:XH], func=AF.Copy), reads=[phb], writes=[hsb])
              c.op("dve", lambda e, ch=ch: e.tensor_tensor(out=gbuf[:, ch, 0:2], in0=pc[:, XH - 2:XH], in1=hs[:, XH - 2:XH], op=ALU.mult),
                   reads=[pcb, hsb], writes=[gbb])
          for i in range(NT if STOP >= 2 else 0):
              col0 = XH + i * TT
              xt, xtb = xr.next()
              c.dma("sp", xt[:], xTv[:, :, col0:col0 + TT], writes=[xtb], sembuf=xtb)
              for ch in range(4):
                  pb_, pbb = pr.next()
                  pc, pcb = pr.next()
                  ph, phb = pr.next()
                  for (pp, ppb, off) in ((pb_, pbb, 0), (pc, pcb, 512), (ph, phb, 1024)):
                      for k in range(16):
                          c.op("pe", lambda e, k=k, pp=pp, off=off, ch=ch: e.matmul(pp[:], w3[:, k, off + 128 * ch:off + 128 * ch + 128], xt[:, k, :], start=(k == 0), stop=(k == 15)),
                               reads=[xtb, w3b], writes=[ppb], pe_chain=(k > 0))
                  c.op("act", lambda e: e.activation(out=hs[:], in_=ph[:], func=AF.Copy), reads=[phb], writes=[hsb])
                  c.op("dve", lambda e, ch=ch: e.tensor_tensor(out=gbuf[:, ch, 2:2 + TT], in0=pc[:], in1=hs[:], op=ALU.mult),
                       reads=[pcb, hsb], writes=[gbb])
                  c.op("dve", lambda e, ch=ch: e.tensor_scalar(out=acc[:], in0=gbuf[:, ch, 0:TT], scalar1=cw[:, 0, ch:ch + 1], scalar2=None, op0=ALU.mult),
                       reads=[gbb, cwb], writes=[accb])
                  c.op("dve", lambda e, ch=ch: e.scalar_tensor_tensor(out=acc[:], in0=gbuf[:, ch, 1:1 + TT], scalar=cw[:, 1, ch:ch + 1], in1=acc[:], op0=ALU.mult, op1=ALU.add),
                       reads=[gbb, cwb, accb], writes=[accb])
                  c.op("dve", lambda e, ch=ch: e.scalar_tensor_tensor(out=acc[:], in0=gbuf[:, ch, 2:2 + TT], scalar=cw[:, 2, ch:ch + 1], in1=acc[:], op0=ALU.mult, op1=ALU.add),
                       reads=[gbb, cwb, accb], writes=[accb])
                  c.op("dve", lambda e, ch=ch: e.tensor_tensor(out=yc[:, ch, :], in0=pb_[:], in1=acc[:], op=ALU.mult), reads=[pbb, accb], writes=[ycb])
                  c.op("act", lambda e, ch=ch: e.activation(out=sq[:, ch, :], in_=yc[:, ch, :], func=AF.Square), reads=[ycb], writes=[sqb])
                  c.op("pool", lambda e, ch=ch: e.tensor_copy(out=gbuf[:, ch, 0:2], in_=gbuf[:, ch, TT:TT + 2]), reads=[gbb], writes=[gbb])
              for ch in range(4 if STOP >= 3 else 0):
                  c.op("pe", lambda e, ch=ch: e.matmul(pss[:], ones[:], sq[:, ch, :], start=(ch == 0), stop=(ch == 3)),
                       reads=[sqb, onesb], writes=[pssb], pe_chain=(ch > 0))
              STOP >= 4 and rstd_from(c, "act", rsF[:], pss[:], 1.0 / 512, ce[:, 0:1], [pssb, ceb], rsFb)
              for ch in range(4 if STOP >= 5 else 0):
                  ob, obb = obr.next()
                  c.op("dve", lambda e, ch=ch: e.scalar_tensor_tensor(out=ob[:], in0=yc[:, ch, :], scalar=gn[:, 4 + ch:5 + ch], in1=rsF[:], op0=ALU.mult, op1=ALU.mult),
                       reads=[ycb, gnb, rsFb], writes=[obb])
                  if STOP >= 6:
                      c.dma("sp", T["mT"][128 * ch:128 * ch + 128, i * TT:(i + 1) * TT], ob[:], reads=[obb], writes=[dm], sembuf=obb)
          c.barrier([b for (_, b) in xr.items + obr.items + pr.items] + [w3b, cwb, gnb, xhb, gbb, hsb, accb, ycb, sqb, rsFb, pssb])
          c.flush()
          c.stack = old

    with contextlib.ExitStack() as st:
      if '3' in PARTS:
          c.stack, old = st, c.stack
          w3, w3b = c.sbuf("w3", [128, 16, 1536], BF16)
          for k0 in range(0, 16, 2):
              c.dma("pool", w3[:, k0:k0 + 2, :], w_in[:, k0:k0 + 2, 2880:4416], writes=[w3b], sembuf=w3b)
          gn, gnb = c.sbuf("gain", [128, 16], F32)
          c.dma("sp", gn[:], T["gain"].rearrange("(c p) -> p c", p=128), writes=[gnb], sembuf=gnb)
          gnbc, gnbcb = c.sbuf("gnbc", [128, 512], F32)
          c.dma("sp", gnbc[:], T["gain"][1536:2048].partition_broadcast(128), writes=[gnbcb], sembuf=gnbcb)
          lg, lgb = c.sbuf("lng", [128, 512], F32)
          lb, lbb = c.sbuf("lnb", [128, 512], F32)
          c.dma("sp", lg[:], T["sgu_ln_g"].partition_broadcast(128), writes=[lgb], sembuf=lgb)
          c.dma("sp", lb[:], T["sgu_ln_b"].partition_broadcast(128), writes=[lbb], sembuf=lbb)
          sb_, sbb = c.sbuf("sgub", [128, 4], F32)
          c.dma("sp", sb_[:], T["sgu_b"].rearrange("g t -> t g"), writes=[sbb], sembuf=sbb, allow_slow_non_contiguous=True)
          pw, pwb = c.sbuf("poolw", [128, 4, 128], BF16)
          c.dma("pool", pw[:], T["pool_w"].rearrange("g c d -> c g d"), writes=[pwb], sembuf=pwb)
          invc, invcb = c.sbuf("invc", [128, 4, XH], F32)
          c.dma("sp", invc[:], T["invc"].partition_broadcast(128), writes=[invcb], sembuf=invcb)
          swf, swfb = c.sbuf("swf", [128, 4, 128], F32)
          c.dma("sp", swf[:], T["sgu_w"].rearrange("g t s -> t g s"), writes=[swfb], sembuf=swfb)
          wsT, wsTb = c.sbuf("wsT", [128, 4, 128], BF16)
          pr = Ring(c, "pA", [128, TT], F32, 4, psum=True)
          pm_r = Ring(c, "pM", [128, TT], F32, 1, psum=True)
          ptr, ptrb = c.psum("ptr", [128, 4, 128], BF16)
          pss, pssb = c.psum("pss", [128, TT], F32)
          for g in range(4):
              c.op("pool", lambda e, g=g: e.affine_select(out=swf[:, g, :], in_=swf[:, g, :], pattern=[[-1, 128]], compare_op=ALU.is_ge,
                                                         fill=0.0, base=0, channel_multiplier=1), reads=[swfb], writes=[swfb])
              pa, pab = pr.next()
              c.op("pe", lambda e, g=g: e.transpose(pa[:, 0:128], swf[:, g, :], idf[:]), reads=[swfb, idfb], writes=[pab])
              c.op("act", lambda e, g=g: e.activation(out=wsT[:, g, :], in_=pa[:, 0:128], func=AF.Copy), reads=[pab], writes=[wsTb])
          xr = Ring(c, "xt", [128, 16, TT], BF16, 2)
          xh, xhb = c.sbuf("xh", [128, 16, XH], BF16)
          hbuf, hbb = c.sbuf("hbuf", [128, 4, XH + TT], F32)
          sa, sab = c.sbuf("sa", [128, XH + TT], F32)
          sb2, sb2b = c.sbuf("sb2", [128, XH + TT], F32)
          pbf, pbfb = c.sbuf("pbf", [128, TT], BF16)
          yp, ypb = c.sbuf("yp", [128, 4, TT], F32)
          sq, sqb = c.sbuf("sq", [128, 4, TT], BF16)
          rsF, rsFb = c.sbuf("rsF", [128, TT], F32)
          obr = Ring(c, "ob", [128, TT], BF16, 4)
          gu, gub = c.sbuf("gu", [128, 512], F32)
          gv, gvb = c.sbuf("gv", [128, 512], F32)
          vnb, vnbb = c.sbuf("vnb", [128, 512], BF16)
          ys, ysb = c.sbuf("ys", [128, 512], F32)
          junk, junkb = c.sbuf("junk", [128, 512], BF16)
          ym, ymb = c.sbuf("ym", [128, 512], BF16)
          msg, msgb = c.sbuf("msg", [128, 4, TT], BF16)
          bn, bnb = c.sbuf("bn", [128, 6], F32)
          sts, stsb = c.sbuf("sts", [128, 8], F32)
          c.dma("sp", xh[:], xTv[:, :, 0:XH], writes=[xhb], sembuf=xhb)
          for g in range(4):
              ph, phb = pr.next()
              for k in range(16):
                  c.op("pe", lambda e, k=k, g=g: e.matmul(ph[:, 0:XH], w3[:, k, 128 * g:128 * g + 128], xh[:, k, :], start=(k == 0), stop=(k == 15)),
                       reads=[xhb, w3b], writes=[phb], pe_chain=(k > 0))
              c.op("act", lambda e, g=g: e.activation(out=hbuf[:, g, 0:XH], in_=ph[:, 0:XH], func=AF.Copy), reads=[phb], writes=[hbb])
          NB = XH + TT
          for i in range(NT):
              col0 = XH + i * TT
              xt, xtb = xr.next()
              c.dma("sp", xt[:], xTv[:, :, col0:col0 + TT], writes=[xtb], sembuf=xtb)
              for g in range(4):
                  ph, phb = pr.next()
                  for k in range(16):
                      c.op("pe", lambda e, k=k, g=g: e.matmul(ph[:], w3[:, k, 128 * g:128 * g + 128], xt[:, k, :], start=(k == 0), stop=(k == 15)),
                           reads=[xtb, w3b], writes=[phb], pe_chain=(k > 0))
                  c.op("act", lambda e, g=g: e.activation(out=hbuf[:, g, XH:NB], in_=ph[:], func=AF.Copy), reads=[phb], writes=[hbb])
                  hb = hbuf[:, g, :]
                  c.op("dve", lambda e, hb=hb: e.tensor_tensor(out=sa[:, 1:NB], in0=hb[:, 1:NB], in1=hb[:, 0:NB - 1], op=ALU.add), reads=[hbb], writes=[sab])
                  S, Sb = sa, sab
                  if g >= 1:
                      c.op("dve", lambda e: e.tensor_tensor(out=sb2[:, 3:NB], in0=sa[:, 3:NB], in1=sa[:, 1:NB - 2], op=ALU.add), reads=[sab], writes=[sb2b])
                      S, Sb = sb2, sb2b
                  if g >= 2:
                      c.op("dve", lambda e: e.tensor_tensor(out=sa[:, 7:NB], in0=sb2[:, 7:NB], in1=sb2[:, 3:NB - 4], op=ALU.add), reads=[sb2b], writes=[sab])
                      S, Sb = sa, sab
                  if g >= 3:
                      c.op("dve", lambda e: e.tensor_tensor(out=sb2[:, 15:NB], in0=sa[:, 15:NB], in1=sa[:, 7:NB - 8], op=ALU.add), reads=[sab], writes=[sb2b])
                      S, Sb = sb2, sb2b
                  wgt = 1.0 / (2 << g)
                  if i == 0:
                      c.op("dve", lambda e, S=S, g=g: e.tensor_tensor(out=S[:, XH:2 * XH], in0=S[:, XH:2 * XH], in1=invc[:, g, :], op=ALU.mult),
                           reads=[Sb, invcb], writes=[Sb])
                      c.op("dve", lambda e, S=S, hb=hb: e.tensor_tensor(out=pbf[:, 0:XH], in0=S[:, XH:2 * XH], in1=hb[:, XH:2 * XH], op=ALU.subtract),
                           reads=[Sb, hbb], writes=[pbfb])
                      c.op("dve", lambda e, S=S, hb=hb, wgt=wgt: e.scalar_tensor_tensor(out=pbf[:, XH:TT], in0=S[:, 2 * XH:NB], scalar=wgt, in1=hb[:, 2 * XH:NB], op0=ALU.mult, op1=ALU.subtract),
                           reads=[Sb, hbb], writes=[pbfb])
                  else:
                      c.op("dve", lambda e, S=S, hb=hb, wgt=wgt: e.scalar_tensor_tensor(out=pbf[:], in0=S[:, XH:NB], scalar=wgt, in1=hb[:, XH:NB], op0=ALU.mult, op1=ALU.subtract),
                           reads=[Sb, hbb], writes=[pbfb])
                  py, pyb = pr.next()
                  c.op("pe", lambda e, g=g: e.matmul(py[:], pw[:, g, :], pbf[:], start=True, stop=True), reads=[pwb, pbfb], writes=[pyb])
                  c.op("act", lambda e, g=g: e.activation(out=yp[:, g, :], in_=py[:], func=AF.Copy), reads=[pyb], writes=[ypb])
                  c.op("act", lambda e, g=g: e.activation(out=sq[:, g, :], in_=py[:], func=AF.Square), reads=[pyb], writes=[sqb])
                  c.op("pool", lambda e, g=g: e.tensor_copy(out=hbuf[:, g, 0:XH], in_=hbuf[:, g, TT:NB]), reads=[hbb], writes=[hbb])
              for ch in range(4):
                  c.op("pe", lambda e, ch=ch: e.matmul(pss[:], ones[:], sq[:, ch, :], start=(ch == 0), stop=(ch == 3)),
                       reads=[sqb, onesb], writes=[pssb], pe_chain=(ch > 0))
              rstd_from(c, "act", rsF[:], pss[:], 1.0 / 512, ce[:, 0:1], [pssb, ceb], rsFb)
              for ch in range(4):
                  ob, obb = obr.next()
                  c.op("dve", lambda e, ch=ch: e.scalar_tensor_tensor(out=ob[:], in0=yp[:, ch, :], scalar=gn[:, 8 + ch:9 + ch], in1=rsF[:], op0=ALU.mult, op1=ALU.mult),
                       reads=[ypb, gnb, rsFb], writes=[obb])
                  c.dma("sp", T["mT"][512 + 128 * ch:512 + 128 * ch + 128, i * TT:(i + 1) * TT], ob[:], reads=[obb], writes=[dm], sembuf=obb)
              for sub in range(4):
                  pu, pub = pr.next()
                  pv, pvb = pr.next()
                  for (pp, ppb, off) in ((pu, pub, 512), (pv, pvb, 1024)):
                      for k in range(16):
                          c.op("pe", lambda e, k=k, pp=pp, off=off, sub=sub: e.matmul(pp[:], xt[:, k, sub * 128:(sub + 1) * 128], w3[:, k, off:off + 512], start=(k == 0), stop=(k == 15)),
                               reads=[xtb, w3b], writes=[ppb], pe_chain=(k > 0))
                  c.op("act", lambda e: e.activation(out=gu[:], in_=pu[:], func=AF.Gelu), reads=[pub], writes=[gub])
                  c.op("act", lambda e: e.activation(out=gv[:], in_=pv[:], func=AF.Gelu), reads=[pvb], writes=[gvb])
                  c.op("dve", lambda e: e.bn_stats(out=bn[:], in_=gv[:]), reads=[gvb], writes=[bnb])
                  c.op("dve", lambda e: e.bn_aggr(out=sts[:, 0:2], in_=bn[:]), reads=[bnb], writes=[stsb])
                  rstd_from(c, "act", sts[:, 2:3], sts[:, 1:2], 1.0, ce[:, 1:2], [stsb, ceb], stsb)
                  c.op("dve", lambda e: e.tensor_scalar(out=gv[:], in0=gv[:], scalar1=sts[:, 0:1], scalar2=sts[:, 2:3], op0=ALU.subtract, op1=ALU.mult),
                       reads=[gvb, stsb], writes=[gvb])
                  c.op("dve", lambda e: e.tensor_tensor(out=gv[:], in0=gv[:], in1=lg[:], op=ALU.mult), reads=[gvb, lgb], writes=[gvb])
                  c.op("dve", lambda e: e.tensor_tensor(out=vnb[:], in0=gv[:], in1=lb[:], op=ALU.add), reads=[gvb, lbb], writes=[vnbb])
                  pm, pmb = pm_r.next()
                  for g in range(4):
                      c.op("pe", lambda e, g=g: e.matmul(pm[:, 128 * g:128 * g + 128], wsT[:, g, :], vnb[:, 128 * g:128 * g + 128], start=True, stop=True),
                           reads=[wsTb, vnbb], writes=[pmb], pe_chain=(g > 0))
                  for g in range(4):
                      c.op("dve", lambda e, g=g: e.scalar_tensor_tensor(out=ys[:, 128 * g:128 * g + 128], in0=pm[:, 128 * g:128 * g + 128], scalar=sb_[:, g:g + 1],
                                                                       in1=gu[:, 128 * g:128 * g + 128], op0=ALU.add, op1=ALU.mult),
                           reads=[pmb, sbb, gub], writes=[ysb])
                  c.op("act", lambda e: e.activation(out=junk[:], in_=ys[:], func=AF.Square, accum_out=sts[:, 4:5]), reads=[ysb], writes=[junkb, stsb])
                  rstd_from(c, "act", sts[:, 5:6], sts[:, 4:5], 1.0 / 512, ce[:, 0:1], [stsb, ceb], stsb)
                  c.op("dve", lambda e: e.scalar_tensor_tensor(out=ym[:], in0=ys[:], scalar=sts[:, 5:6], in1=gnbc[:], op0=ALU.mult, op1=ALU.mult),
                       reads=[ysb, stsb, gnbcb], writes=[ymb])
                  for k in range(4):
                      c.op("pe", lambda e, k=k: e.transpose(ptr[:, k, :], ym[:, 128 * k:128 * k + 128], idh[:]), reads=[ymb, idhb], writes=[ptrb], pe_chain=(k > 0))
                  c.op("act", lambda e, sub=sub: e.activation(out=msg[:, :, sub * 128:(sub + 1) * 128], in_=ptr[:], func=AF.Copy), reads=[ptrb], writes=[msgb])
              c.dma("sp", T["mT"][1024:1536, i * TT:(i + 1) * TT].rearrange("(c p) t -> p c t", p=128), msg[:], reads=[msgb], writes=[dm], sembuf=msgb)
          c.barrier([b for (_, b) in xr.items + obr.items + pr.items + pm_r.items] + [w3b, gnb, gnbcb, lgb, lbb, sbb, pwb, invcb, swfb, wsTb, ptrb, pssb, xhb, hbb, sab, sb2b, pbfb, ypb, sqb, rsFb, gub, gvb, vnbb, ysb, junkb, ymb, msgb, bnb, stsb])
          c.flush()
          c.stack = old
    return [dq, dk, dv, dm]

import contextlib, math
import numpy as np

ALPHA = (2.0 * 4) ** 0.25
ATT_SCALE = 1.0 / math.sqrt(192.0)
DFF = 5632
NF = DFF // 128


def phase_b1a(c, K, NT, NPAST, T):
    ones, onesb = K["ones"]
    ce, ceb = K["ce"]
    dm0 = Buf("dram_m0")
    with contextlib.ExitStack() as st:
        c.stack, old = st, c.stack
        K["pcol"] = c.psum("pcol", [128, 16], F32)
        gn, gnb = load_cols(c, K, "gain", T["gain"].rearrange("(c p) -> c p", p=128), 16)
        qb, qbb = c.sbuf("qb", [128, 4], F32)
        c.dma("sp", qb[:, 0:max(NPAST, 1)], T["qbias"], writes=[qbb], sembuf=qbb)
        qnr = Ring(c, "qn", [128, 2, TT], BF16, 2)
        qrr = Ring(c, "qr", [64, 2, TT], BF16, 2)
        knr = Ring(c, "kn", [128, 2, 512], BF16, 3)
        krr = Ring(c, "kr", [64, 512], BF16, 3)
        vvr = Ring(c, "vv", [128, 4, 256], BF16, 3)
        ptr_ = Ring(c, "pT", [128, TT], BF16, 4)
        ya, yab = c.sbuf("ya", [128, 4, TT], F32)
        sq, sqb = c.sbuf("sq", [128, 4, TT], BF16)
        rl, rlb = c.sbuf("rl", [128, TT], F32)
        rsF, rsFb = c.sbuf("rsF", [128, TT], F32)
        obr = Ring(c, "ob", [128, TT], BF16, 4)
        Sr = Ring(c, "pS", [128, TT], F32, 3, psum=True)
        Or = [c.psum("pO%d" % k, [128, TT], F32) for k in range(2)]
        Lr = [c.psum("pL%d" % k, [128, TT], F32) for k in range(2)]
        for i in range(NT):
            cs = slice(i * TT, (i + 1) * TT)
            for hp in range(2):
                qn, qnb = qnr.next()
                qr, qrb = qrr.next()
                c.dma("sp", qn[:], T["QT"][2 * hp:2 * hp + 2, 0:128, cs].rearrange("h d t -> d h t"), writes=[qnb], sembuf=qnb)
                c.dma("sp", qr[:], T["QT"][2 * hp:2 * hp + 2, 128:192, cs].rearrange("h d t -> d h t"), writes=[qrb], sembuf=qrb)
                chunks = [(j, cc) for j in range(NPAST) for cc in range(NT)] + [(None, cc) for cc in range(i + 1)]
                nblk = len(chunks) * 4
                bi = 0
                for (j, cc) in chunks:
                    kc = slice(cc * 512, (cc + 1) * 512)
                    KTs = T["KTo"] if j is None else T["KTp"][j]
                    Vs = T["Vo"] if j is None else T["Vp"][j]
                    kn, knb = knr.next(); kr, krb = krr.next(); vv, vvb = vvr.next()
                    c.dma("sp", kn[:], KTs[256 * hp:256 * hp + 256, kc].rearrange("(h d) t -> d h t", h=2), writes=[knb], sembuf=knb)
                    c.dma("sp", kr[:], KTs[512:576, kc], writes=[krb], sembuf=krb)
                    c.dma("sp", vv[:], Vs[kc, 256 * hp:256 * hp + 256].rearrange("(b p) d -> p b d", p=128), writes=[vvb], sembuf=vvb)
                    diag = (j is None and cc == i)
                    bias = ce[:, 2:3] if j is None else qb[:, j:j + 1]
                    for kb in range(4):
                        q0 = 128 * kb if diag else 0
                        for hh in range(2):
                            ps, psb = Sr.next()
                            c.op("pe", lambda e: e.matmul(ps[:, q0:TT], kn[:, hh, kb * 128:(kb + 1) * 128], qn[:, hh, q0:TT], start=True, stop=False),
                                 reads=[knb, qnb], writes=[psb])
                            c.op("pe", lambda e: e.matmul(ps[:, q0:TT], kr[:, kb * 128:(kb + 1) * 128], qr[:, hh, q0:TT], start=False, stop=True),
                                 reads=[krb, qrb], writes=[psb], pe_chain=True)
                            pT, pTb = ptr_.next()
                            c.op("act", lambda e: e.activation(out=pT[:, q0:TT], in_=ps[:, q0:TT], func=AF.Exp, bias=bias, scale=ATT_SCALE),
                                 reads=[psb, qbb, ceb], writes=[pTb])
                            if diag:
                                c.op("pool", lambda e: e.memset(pT[64:128, q0:q0 + 64], 0.0), reads=[], writes=[pTb])
                            (O, Ob), (L, Lb) = Or[hh], Lr[hh]
                            c.op("pe", lambda e: e.matmul(O[:, q0:TT], vv[:, kb, hh * 128:(hh + 1) * 128], pT[:, q0:TT], start=(bi == 0), stop=(bi == nblk - 1)),
                                 reads=[vvb, pTb], writes=[Ob], pe_chain=(bi > 0))
                            c.op("pe", lambda e: e.matmul(L[:, q0:TT], ones[:], pT[:, q0:TT], start=(bi == 0), stop=(bi == nblk - 1)),
                                 reads=[onesb, pTb], writes=[Lb], pe_chain=(bi > 0))
                        bi += 1
                for hh in range(2):
                    (O, Ob), (L, Lb) = Or[hh], Lr[hh]
                    c.op("dve", lambda e: e.reciprocal(out=rl[:], in_=L[:]), reads=[Lb], writes=[rlb])
                    c.op("dve", lambda e: e.tensor_tensor(out=ya[:, 2 * hp + hh, :], in0=O[:], in1=rl[:], op=ALU.mult), reads=[Ob, rlb], writes=[yab])
            for h in range(4):
                c.op("act", lambda e: e.activation(out=sq[:, h, :], in_=ya[:, h, :], func=AF.Square), reads=[yab], writes=[sqb])
            pss, pssb = Sr.next()
            for h in range(4):
                c.op("pe", lambda e: e.matmul(pss[:], ones[:], sq[:, h, :], start=(h == 0), stop=(h == 3)), reads=[sqb, onesb], writes=[pssb], pe_chain=(h > 0))
            rstd_from(c, "act", rsF[:], pss[:], 1.0 / 512, ce[:, 0:1], [pssb, ceb], rsFb)
            for h in range(4):
                ob, obb = obr.next()
                c.op("dve", lambda e: e.scalar_tensor_tensor(out=ob[:], in0=ya[:, h, :], scalar=gn[:, h:h + 1], in1=rsF[:], op0=ALU.mult, op1=ALU.mult),
                     reads=[yab, gnb, rsFb], writes=[obb])
                c.dma("sp", T["mT0"][128 * h:128 * h + 128, cs], ob[:], reads=[obb], writes=[dm0], sembuf=obb)
        allb = [b for r in (qnr, qrr, knr, krr, vvr, ptr_, obr, Sr) for (_, b) in r.items] + [b for (_, b) in Or + Lr] + [gnb, qbb, yab, sqb, rlb, rsFb, K["pcol"][1]]
        c.barrier(allb)
        c.flush()
        c.stack = old
    return [dm0]


def layer_norm_rows(c, z, zb, bn, bnb, sts, stsb, gt, gtb, bt, btb, ce, ceb, out, outb):
    for k in range(4):
        c.op("dve", lambda e: e.bn_stats(out=bn[:, k, :], in_=z[:, 512 * k:512 * k + 512]), reads=[zb], writes=[bnb])
    c.op("dve", lambda e: e.bn_aggr(out=sts[:, 0:2], in_=bn[:].rearrange("p a b -> p (a b)")), reads=[bnb], writes=[stsb])
    rstd_from(c, "act", sts[:, 2:3], sts[:, 1:2], 1.0, ce[:, 1:2], [stsb, ceb], stsb)
    c.op("dve", lambda e: e.tensor_scalar(out=z[:], in0=z[:], scalar1=sts[:, 0:1], scalar2=sts[:, 2:3], op0=ALU.subtract, op1=ALU.mult),
         reads=[zb, stsb], writes=[zb])
    c.op("pool", lambda e: e.tensor_tensor(out=z[:], in0=z[:], in1=gt[:], op=ALU.mult), reads=[zb, gtb], writes=[zb])
    c.op("dve", lambda e: e.tensor_tensor(out=out[:], in0=z[:], in1=bt[:], op=ALU.add), reads=[zb, btb], writes=[outb])


def transpose_out(c, K, x1, x1b, pr, xTt, xTtb, x32=None):
    idf, idfb = K["idf"]
    for q in range(4):
        pt, ptb = pr.next()
        for k in range(4):
            cidx = 4 * q + k
            c.op("pe", lambda e: e.transpose(pt[:, 128 * k:128 * k + 128], x1[:, 128 * cidx:128 * cidx + 128], idf[:]), reads=[x1b, idfb], writes=[ptb], pe_chain=(k > 0))
        c.op("act", lambda e: e.activation(out=xTt[:, 4 * q:4 * q + 4, :], in_=pt[:].rearrange("p (k t) -> p k t", k=4), func=AF.Copy), reads=[ptb], writes=[xTtb])
        if x32 is not None:
            c.op("dve", lambda e: e.tensor_copy(out=x32[0][:, 4 * q:4 * q + 4, :], in_=pt[:].rearrange("p (k t) -> p k t", k=4)), reads=[ptb], writes=[x32[1]])


def phase_b1b(c, K, NT, T, moe):
    ce, ceb = K["ce"]
    dx1 = Buf("dram_x1"); dx1T = Buf("dram_x1T"); dlg = Buf("dram_lg")
    with contextlib.ExitStack() as st:
        c.stack, old = st, c.stack
        wo, wob = c.sbuf("wo", [128, 16, 2048], BF16)
        wov = T["w_o"].rearrange("(c p) n -> p c n", p=128)
        for k0 in range(0, 16, 2):
            c.dma("pool", wo[:, k0:k0 + 2, :], wov[:, k0:k0 + 2, :], writes=[wob], sembuf=wob)
        g1, g1b = c.sbuf("g1", [128, 2048], F32)
        b1, b1b = c.sbuf("b1", [128, 2048], F32)
        c.dma("sp", g1[:], T["ln_g"].partition_broadcast(128), writes=[g1b], sembuf=g1b)
        c.dma("sp", b1[:], T["ln_b"].partition_broadcast(128), writes=[b1b], sembuf=b1b)
        if moe:
            rw, rwb = c.sbuf("rw", [128, 16, 8], F32)
            c.dma("sp", rw[:], T["router_w"].rearrange("(c p) e -> p c e", p=128), writes=[rwb], sembuf=rwb)
            x32 = c.sbuf("x32", [128, 16, 128], F32)
            lgt, lgtb = c.sbuf("lgt", [128, 8], F32)
        mtr = Ring(c, "mt", [128, 16, TT], BF16, 2)
        xsr = Ring(c, "xs", [128, 2048], F32, 2)
        z, zb = c.sbuf("z", [128, 2048], F32)
        x1r = Ring(c, "x1", [128, 2048], F32, 2)
        xTr = Ring(c, "xTt", [128, 16, 128], BF16, 2)
        bn, bnb = c.sbuf("bn", [128, 4, 6], F32)
        sts, stsb = c.sbuf("sts", [128, 8], F32)
        pr = Ring(c, "pA", [128, 512], F32, 6, psum=True)
        plg, plgb = c.psum("plg", [128, 8], F32)
        for i in range(NT):
            cs = slice(i * TT, (i + 1) * TT)
            mt, mtb = mtr.next()
            c.dma("sp", mt[:, 0:4, :], T["mT0"][:, cs].rearrange("(c p) t -> p c t", p=128), writes=[mtb], sembuf=mtb)
            c.dma("sp", mt[:, 4:16, :], T["mT"][:, cs].rearrange("(c p) t -> p c t", p=128), writes=[mtb], sembuf=mtb)
            for sub in range(4):
                t0 = i * TT + sub * 128
                xs, xsb = xsr.next()
                c.dma("sp", xs[:], T["x"][t0:t0 + 128, :], writes=[xsb], sembuf=xsb)
                for n in range(4):
                    pp, ppb = pr.next()
                    for k in range(16):
                        c.op("pe", lambda e: e.matmul(pp[:], mt[:, k, sub * 128:(sub + 1) * 128], wo[:, k, 512 * n:512 * n + 512], start=(k == 0), stop=(k == 15)),
                             reads=[mtb, wob], writes=[ppb], pe_chain=(k > 0))
                    c.op("dve", lambda e: e.scalar_tensor_tensor(out=z[:, 512 * n:512 * n + 512], in0=xs[:, 512 * n:512 * n + 512], scalar=ALPHA, in1=pp[:], op0=ALU.mult, op1=ALU.add),
                         reads=[xsb, ppb], writes=[zb])
                x1, x1b = x1r.next()
                layer_norm_rows(c, z, zb, bn, bnb, sts, stsb, g1, g1b, b1, b1b, ce, ceb, x1, x1b)
                c.dma("sp", T["x1"][t0:t0 + 128, :], x1[:], reads=[x1b], writes=[dx1], sembuf=x1b)
                xTt, xTtb = xTr.next()
                transpose_out(c, K, x1, x1b, pr, xTt, xTtb, x32 if moe else None)
                c.dma("sp", T["x1T"][:, t0:t0 + 128].rearrange("(c p) t -> p c t", p=128), xTt[:], reads=[xTtb], writes=[dx1T], sembuf=xTtb)
                if moe:
                    for k in range(16):
                        c.op("pe", lambda e: e.matmul(plg[:], x32[0][:, k, :], rw[:, k, :], start=(k == 0), stop=(k == 15)), reads=[x32[1], rwb], writes=[plgb], pe_chain=(k > 0))
                    c.op("dve", lambda e: e.tensor_copy(out=lgt[:], in_=plg[:]), reads=[plgb], writes=[lgtb])
                    c.dma("sp", T["logits"][t0:t0 + 128, :], lgt[:], reads=[lgtb], writes=[dlg], sembuf=lgtb)
        allb = [b for r in (mtr, xsr, x1r, xTr, pr) for (_, b) in r.items] + [wob, g1b, b1b, zb, bnb, stsb, plgb]
        if moe:
            allb += [rwb, x32[1], lgtb]
        c.barrier(allb)
        c.flush()
        c.stack = old
    return [dx1, dx1T, dlg]


def phase_b2(c, K, NT, T, moe):
    ce, ceb = K["ce"]
    idf, idfb = K["idf"]
    dxo = Buf("dram_xo"); dxoT = Buf("dram_xoT")
    NE = 8 if moe else 1
    with contextlib.ExitStack() as st:
        c.stack, old = st, c.stack
        g2, g2b = c.sbuf("g2", [128, 2048], F32)
        b2, b2b = c.sbuf("b2", [128, 2048], F32)
        c.dma("sp", g2[:], T["ln_g"].partition_broadcast(128), writes=[g2b], sembuf=g2b)
        c.dma("sp", b2[:], T["ln_b"].partition_broadcast(128), writes=[b2b], sembuf=b2b)
        xt, xtb = c.sbuf("xt", [128, 16, TT], BF16)
        hT, hTb = c.sbuf("hT", [128, NF, TT], BF16)
        wgr = Ring(c, "wgf", [128, 16, 128], F32, 2)
        wur = Ring(c, "wuf", [128, 16, 128], F32, 2)
        wgbr = Ring(c, "wgb", [128, 16, 128], BF16, 2)
        wubr = Ring(c, "wub", [128, 16, 128], BF16, 2)
        wdr = Ring(c, "wdf", [128, 1024], F32, 2)
        wdbr = Ring(c, "wdb", [128, 1024], BF16, 2)
        sg, sgb = c.sbuf("sg", [128, TT], F32)
        z, zb = c.sbuf("z", [128, 4, 2048], F32)
        x1r = Ring(c, "xo", [128, 2048], F32, 1)
        xTr = Ring(c, "xTt", [128, 16, 128], BF16, 1)
        bn, bnb = c.sbuf("bn", [128, 4, 6], F32)
        sts, stsb = c.sbuf("sts", [128, 16], F32)
        if moe:
            lg, lgb = c.sbuf("lg", [128, 8], F32)
            mx, mxb = c.sbuf("mx", [128, 8], F32)
            gt, gtb = c.sbuf("gt", [128, 8], F32)
            gT, gTb = c.sbuf("gT", [8, TT], F32)
            sel, selb = c.sbuf("sel", [8, 8, 128], F32)
            gbc, gbcb = c.sbuf("gbc", [128, 8, TT], BF16)
            c.op("pool", lambda e: e.memset(sel[:], 1.0), writes=[selb])
            c.op("pool", lambda e: e.affine_select(out=sel[:], in_=sel[:], pattern=[[-1, 8], [0, 128]], compare_op=ALU.is_equal, fill=0.0, base=0, channel_multiplier=1),
                 reads=[selb], writes=[selb])
        pr = Ring(c, "pA", [128, 512], F32, 8, psum=True)
        for i in range(NT):
            cs = slice(i * TT, (i + 1) * TT)
            c.dma("sp", xt[:], T["x1T"][:, cs].rearrange("(c p) t -> p c t", p=128), writes=[xtb], sembuf=xtb)
            for sub in range(4):
                t0 = i * TT + sub * 128
                c.dma("sp", z[:, sub, :], T["x1"][t0:t0 + 128, :], writes=[zb], sembuf=zb)
            for sub in range(4):
                c.op("act", lambda e: e.activation(out=z[:, sub, :], in_=z[:, sub, :], func=AF.Copy, scale=ALPHA), reads=[zb], writes=[zb])
            if moe:
                for sub in range(4):
                    t0 = i * TT + sub * 128
                    c.dma("sp", lg[:], T["logits"][t0:t0 + 128, :], writes=[lgb], sembuf=lgb)
                    c.op("dve", lambda e: e.max(out=mx[:], in_=lg[:]), reads=[lgb], writes=[mxb])
                    c.op("dve", lambda e: e.tensor_scalar(out=gt[:], in0=lg[:], scalar1=mx[:, 1:2], scalar2=None, op0=ALU.is_ge), reads=[lgb, mxb], writes=[gtb])
                    c.op("dve", lambda e: e.tensor_scalar(out=sts[:, 8:9], in0=mx[:, 0:1], scalar1=-1.0, scalar2=None, op0=ALU.mult), reads=[mxb], writes=[stsb])
                    c.op("act", lambda e: e.activation(out=lg[:], in_=lg[:], func=AF.Exp, bias=sts[:, 8:9], scale=1.0), reads=[lgb, stsb], writes=[lgb])
                    c.op("dve", lambda e: e.tensor_tensor(out=gt[:], in0=gt[:], in1=lg[:], op=ALU.mult), reads=[gtb, lgb], writes=[gtb])
                    c.op("dve", lambda e: e.tensor_reduce(out=sts[:, 9:10], in_=gt[:], axis=AX.X, op=ALU.add), reads=[gtb], writes=[stsb])
                    c.op("dve", lambda e: e.reciprocal(out=sts[:, 10:11], in_=sts[:, 9:10]), reads=[stsb], writes=[stsb])
                    c.op("dve", lambda e: e.tensor_scalar(out=gt[:], in0=gt[:], scalar1=sts[:, 10:11], scalar2=None, op0=ALU.mult), reads=[gtb, stsb], writes=[gtb])
                    pp, ppb = pr.next()
                    c.op("pe", lambda e: e.transpose(pp[0:8, 0:128], gt[:], idf[:]), reads=[gtb, idfb], writes=[ppb])
                    c.op("act", lambda e: e.activation(out=gT[:, sub * 128:(sub + 1) * 128], in_=pp[0:8, 0:128], func=AF.Copy), reads=[ppb], writes=[gTb])
                for ex in range(8):
                    pp, ppb = pr.next()
                    c.op("pe", lambda e: e.matmul(pp[:], sel[:, ex, :], gT[:], start=True, stop=True), reads=[selb, gTb], writes=[ppb])
                    c.op("act", lambda e: e.activation(out=gbc[:, ex, :], in_=pp[:], func=AF.Copy), reads=[ppb], writes=[gbcb])
            for ex in range(NE):
                wgv = (T["wg"][ex] if moe else T["wg"]).rearrange("(c p) f -> p c f", p=128)
                wuv = (T["wu"][ex] if moe else T["wu"]).rearrange("(c p) f -> p c f", p=128)
                wdv = (T["wd"][ex] if moe else T["wd"])
                for f in range(NF):
                    fs = slice(f * 128, (f + 1) * 128)
                    wgf, wgfb = wgr.next(); wuf, wufb = wur.next()
                    c.dma("sp", wgf[:], wgv[:, :, fs], writes=[wgfb], sembuf=wgfb)
                    c.dma("sp", wuf[:], wuv[:, :, fs], writes=[wufb], sembuf=wufb)
                    wgb_, wgbb = wgbr.next(); wub_, wubb = wubr.next()
                    c.op("act", lambda e: e.activation(out=wgb_[:], in_=wgf[:], func=AF.Copy), reads=[wgfb], writes=[wgbb])
                    c.op("pool", lambda e: e.tensor_copy(out=wub_[:], in_=wuf[:]), reads=[wufb], writes=[wubb])
                    pg, pgb = pr.next(); pu, pub = pr.next()
                    for k in range(16):
                        c.op("pe", lambda e: e.matmul(pg[:], wgb_[:, k, :], xt[:, k, :], start=(k == 0), stop=(k == 15)), reads=[wgbb, xtb], writes=[pgb], pe_chain=(k > 0))
                    for k in range(16):
                        c.op("pe", lambda e: e.matmul(pu[:], wub_[:, k, :], xt[:, k, :], start=(k == 0), stop=(k == 15)), reads=[wubb, xtb], writes=[pub], pe_chain=(k > 0))
                    c.op("act", lambda e: e.activation(out=sg[:], in_=pg[:], func=AF.Silu), reads=[pgb], writes=[sgb])
                    if moe:
                        c.op("dve", lambda e: e.tensor_tensor(out=sg[:], in0=sg[:], in1=gbc[:, ex, :], op=ALU.mult), reads=[sgb, gbcb], writes=[sgb])
                    c.op("dve", lambda e: e.tensor_tensor(out=hT[:, f, :], in0=pu[:], in1=sg[:], op=ALU.mult), reads=[pub, sgb], writes=[hTb])
                for nh in range(2):
                    accs = [pr.next() for _ in range(8)]
                    for f in range(NF):
                        wdf, wdfb = wdr.next()
                        c.dma("sp", wdf[:], wdv[f * 128:(f + 1) * 128, nh * 1024:(nh + 1) * 1024], writes=[wdfb], sembuf=wdfb)
                        wdb_, wdbb = wdbr.next()
                        c.op("dve" if f % 2 == 0 else "pool", lambda e: e.tensor_copy(out=wdb_[:], in_=wdf[:]), reads=[wdfb], writes=[wdbb])
                        for sub in range(4):
                            for n2 in range(2):
                                pa_, pab = accs[sub * 2 + n2]
                                c.op("pe", lambda e: e.matmul(pa_[:], hT[:, f, sub * 128:(sub + 1) * 128], wdb_[:, n2 * 512:(n2 + 1) * 512], start=(f == 0), stop=(f == NF - 1)),
                                     reads=[hTb, wdbb], writes=[pab], pe_chain=(f > 0))
                    for sub in range(4):
                        for n2 in range(2):
                            pa_, pab = accs[sub * 2 + n2]
                            col = nh * 1024 + n2 * 512
                            c.op("dve", lambda e: e.tensor_tensor(out=z[:, sub, col:col + 512], in0=z[:, sub, col:col + 512], in1=pa_[:], op=ALU.add), reads=[zb, pab], writes=[zb])
            for sub in range(4):
                t0 = i * TT + sub * 128
                xo, xob = x1r.next()
                zs = z[:, sub, :]
                layer_norm_rows(c, zs, zb, bn, bnb, sts, stsb, g2, g2b, b2, b2b, ce, ceb, xo, xob)
                c.dma("sp", T["xo"][t0:t0 + 128, :], xo[:], reads=[xob], writes=[dxo], sembuf=xob)
                xTt, xTtb = xTr.next()
                transpose_out(c, K, xo, xob, pr, xTt, xTtb)
                c.dma("sp", T["xoT"][:, t0:t0 + 128].rearrange("(c p) t -> p c t", p=128), xTt[:], reads=[xTtb], writes=[dxoT], sembuf=xTtb)
        allb = [b for r in (wgr, wur, wgbr, wubr, wdr, wdbr, x1r, xTr, pr) for (_, b) in r.items] + [g2b, b2b, xtb, hTb, sgb, zb, bnb, stsb]
        if moe:
            allb += [lgb, mxb, gtb, gTb, selb, gbcb]
        c.barrier(allb)
        c.flush()
        c.stack = old
    return [dxo, dxoT]


import ml_dtypes
NCORE = 8
NTQ = 8
NTOKC = NTQ * TT


def _di(nc, name, shape, dt):
    return nc.dram_tensor(name, list(shape), dt, kind="ExternalInput").ap()


def _do(nc, name, shape, dt):
    return nc.dram_tensor(name, list(shape), dt, kind="ExternalOutput").ap()


def _dint(nc, name, shape, dt):
    return nc.dram_tensor(name, list(shape), dt).ap()


def build_p0():
    nc = bass.Bass("TRN2", target_bir_lowering=False)
    x = _di(nc, "x", [NTOKC, 2048], F32)
    xT = _do(nc, "xT", [2048, NTOKC], BF16)
    with contextlib.ExitStack() as st:
        c = Ctx(nc, st)
        K = consts(c)
        xr = Ring(c, "xs", [128, 2048], F32, 2)
        xTr = Ring(c, "xTt", [128, 16, 128], BF16, 2)
        pr = Ring(c, "pA", [128, 512], F32, 4, psum=True)
        d = Buf("dram_xT")
        for t in range(NTOKC // 128):
            xs, xsb = xr.next()
            c.dma("sp", xs[:], x[t * 128:(t + 1) * 128, :], writes=[xsb], sembuf=xsb)
            xTt, xTtb = xTr.next()
            transpose_out(c, K, xs, xsb, pr, xTt, xTtb)
            c.dma("sp", xT[:, t * 128:(t + 1) * 128].rearrange("(c p) t -> p c t", p=128), xTt[:], reads=[xTtb], writes=[d], sembuf=xTtb)
        c.finish([d])
    return nc


def build_pa():
    nc = bass.Bass("TRN2", target_bir_lowering=False)
    T = dict(
        xT=_di(nc, "xT", [2048, XH + NTOKC], BF16), pos=_di(nc, "pos", [NTOKC], I32), invf=_di(nc, "invf", [64], F32),
        w_in=_di(nc, "w_in", [2048, 4416], F32), kvg=_di(nc, "kvg", [512], F32), w_ukv=_di(nc, "w_ukv", [512, 1024], F32),
        conv_w=_di(nc, "conv_w", [3, 512], F32), pool_w=_di(nc, "pool_w", [4, 128, 128], F32),
        sgu_ln_g=_di(nc, "sgu_ln_g", [512], F32), sgu_ln_b=_di(nc, "sgu_ln_b", [512], F32),
        sgu_w=_di(nc, "sgu_w", [4, 128, 128], F32), sgu_b=_di(nc, "sgu_b", [4, 128], F32), gain=_di(nc, "gain", [2048], F32),
        invc=_di(nc, "invc", [4, XH], F32),
        QT=_do(nc, "QT", [4, 192, NTOKC], BF16), KT=_do(nc, "KT", [576, NTOKC], BF16), V=_do(nc, "V", [NTOKC, 512], BF16),
        mT=_do(nc, "mT", [1536, NTOKC], BF16),
    )
    with contextlib.ExitStack() as st:
        c = Ctx(nc, st)
        K = consts(c)
        outs = phase_a(c, K, NTQ, T)
        c.finish(outs)
    return nc


def build_pb(moe):
    nc = bass.Bass("TRN2", target_bir_lowering=False)
    T = dict(
        QT=_di(nc, "QT", [4, 192, NTOKC], BF16), KTo=_di(nc, "KTo", [576, NTOKC], BF16), Vo=_di(nc, "Vo", [NTOKC, 512], BF16),
        KTp=_di(nc, "KTp", [3, 576, NTOKC], BF16), Vp=_di(nc, "Vp", [3, NTOKC, 512], BF16),
        qbias=_di(nc, "qbias", [128, 3], F32), gain=_di(nc, "gain", [2048], F32),
        mT=_di(nc, "mT", [1536, NTOKC], BF16), x=_di(nc, "x", [NTOKC, 2048], F32), w_o=_di(nc, "w_o", [2048, 2048], F32),
        mT0=_dint(nc, "mT0", [512, NTOKC], BF16), x1=_dint(nc, "x1", [NTOKC, 2048], F32), x1T=_dint(nc, "x1T", [2048, NTOKC], BF16),
        logits=_dint(nc, "logits", [NTOKC, 8], F32), xo=_do(nc, "xo", [NTOKC, 2048], F32), xoT=_do(nc, "xoT", [2048, NTOKC], BF16),
    )
    ln1g = _di(nc, "ln1g", [2048], F32); ln1b = _di(nc, "ln1b", [2048], F32)
    ln2g = _di(nc, "ln2g", [2048], F32); ln2b = _di(nc, "ln2b", [2048], F32)
    if moe:
        rw = _di(nc, "router_w", [2048, 8], F32)
        wg = _di(nc, "wg", [8, 2048, DFF], F32); wu = _di(nc, "wu", [8, 2048, DFF], F32); wd = _di(nc, "wd", [8, DFF, 2048], F32)
    else:
        rw = None
        wg = _di(nc, "wg", [2048, DFF], F32); wu = _di(nc, "wu", [2048, DFF], F32); wd = _di(nc, "wd", [DFF, 2048], F32)
    with contextlib.ExitStack() as st:
        c = Ctx(nc, st)
        K = consts(c)
        outs = []
        outs += phase_b1a(c, K, NTQ, 3, T)
        outs += phase_b1b(c, K, NTQ, dict(T, ln_g=ln1g, ln_b=ln1b, router_w=rw), moe)
        outs += phase_b2(c, K, NTQ, dict(T, ln_g=ln2g, ln_b=ln2b, wg=wg, wu=wu, wd=wd), moe)
        c.finish(outs)
    return nc


def _run(nc, in_maps):
    res = run_bass_kernel_spmd(nc, in_maps, core_ids=list(range(NCORE)))
    return res.results


def kernel(x, positions, w_in, kv_norm_g, w_ukv, conv_w, pool_w, sgu_ln_g, sgu_ln_b, sgu_w, sgu_b,
           mix_gain, w_o, ln1_g, ln1_b, ffn_wg, ffn_wu, ffn_wd, router_w, exp_wg, exp_wu, exp_wd, ln2_g, ln2_b):
    A = np.ascontiguousarray
    x = np.asarray(x, np.float32)
    positions = np.asarray(positions).astype(np.int32)
    bfz = ml_dtypes.bfloat16
    cores = [(c // 4, c % 4) for c in range(NCORE)]
    xs = [A(x[b, r * NTOKC:(r + 1) * NTOKC]) for (b, r) in cores]
    pos = [A(positions[b, r * NTOKC:(r + 1) * NTOKC]) for (b, r) in cores]
    invf = (10000.0 ** (-np.arange(0, 64, 2, dtype=np.float32) / 64)).astype(np.float32)
    invf = np.concatenate([invf, invf]).astype(np.float32)
    invc, qbias = [], []
    for (b, r) in cores:
        t_abs = r * NTOKC + np.arange(XH)
        invc.append(np.stack([1.0 / np.minimum(t_abs + 1, w) for w in (2, 4, 8, 16)]).astype(np.float32))
        qbias.append(np.broadcast_to(np.where(np.arange(3) < r, 0.0, -30000.0).astype(np.float32)[None, :], (128, 3)).copy())
    nc0 = build_p0()
    r0 = _run(nc0, [{"x": xs[c]} for c in range(NCORE)])
    xT = [np.asarray(r0[c]["xT"]) for c in range(NCORE)]
    nca = build_pa()
    ncb = {False: build_pb(False), True: build_pb(True)}
    for l in range(4):
        f = lambda a: A(np.asarray(a[l], np.float32))
        xTh = []
        for c, (b, r) in enumerate(cores):
            halo = xT[c - 1][:, -XH:] if r > 0 else np.zeros((2048, XH), bfz)
            xTh.append(A(np.concatenate([halo, xT[c]], axis=1)))
        wl = dict(w_in=f(w_in), kvg=f(kv_norm_g), w_ukv=f(w_ukv), conv_w=f(conv_w), pool_w=f(pool_w), sgu_ln_g=f(sgu_ln_g),
                  sgu_ln_b=f(sgu_ln_b), sgu_w=f(sgu_w), sgu_b=f(sgu_b), gain=f(mix_gain), invf=invf)
        ra = _run(nca, [dict(wl, xT=xTh[c], pos=pos[c], invc=invc[c]) for c in range(NCORE)])
        del xTh
        KTp, Vp = [], []
        for b in range(2):
            KTp.append(A(np.stack([np.asarray(ra[4 * b + j]["KT"]) for j in range(3)])))
            Vp.append(A(np.stack([np.asarray(ra[4 * b + j]["V"]) for j in range(3)])))
        moe = (l % 2 == 1)
        wb = dict(gain=f(mix_gain), w_o=f(w_o), ln1g=f(ln1_g), ln1b=f(ln1_b), ln2g=f(ln2_g), ln2b=f(ln2_b))
        if moe:
            wb.update(router_w=A(np.asarray(router_w[l // 2], np.float32)), wg=A(np.asarray(exp_wg[l // 2], np.float32)),
                      wu=A(np.asarray(exp_wu[l // 2], np.float32)), wd=A(np.asarray(exp_wd[l // 2], np.float32)))
        else:
            wb.update(wg=A(np.asarray(ffn_wg[l // 2], np.float32)), wu=A(np.asarray(ffn_wu[l // 2], np.float32)),
                      wd=A(np.asarray(ffn_wd[l // 2], np.float32)))
        rb = _run(ncb[moe], [dict(wb, QT=np.asarray(ra[c]["QT"]), KTo=np.asarray(ra[c]["KT"]), Vo=np.asarray(ra[c]["V"]),
                                  KTp=KTp[c // 4], Vp=Vp[c // 4], qbias=qbias[c], mT=np.asarray(ra[c]["mT"]), x=xs[c])
                             for c in range(NCORE)])
        xs = [np.asarray(rb[c]["xo"]) for c in range(NCORE)]
        xT = [np.asarray(rb[c]["xoT"]) for c in range(NCORE)]
        del ra, rb, KTp, Vp
    out = np.empty((2, 4 * NTOKC, 2048), np.float32)
    for c, (b, r) in enumerate(cores):
        out[b, r * NTOKC:(r + 1) * NTOKC] = xs[c]
    return out
```

```python
import contextlib
import numpy as np
import concourse.bass as bass
import concourse.mybir as mybir
from concourse.bass_utils import run_bass_kernel_spmd

F32 = mybir.dt.float32
BF16 = mybir.dt.bfloat16
I32 = mybir.dt.int32
U32 = mybir.dt.uint32
AF = mybir.ActivationFunctionType
ALU = mybir.AluOpType
AX = mybir.AxisListType


class Buf:
    __slots__ = ("name", "last_w", "readers", "dsem", "psum")

    def __init__(self, name):
        self.name = name
        self.last_w = None
        self.readers = []
        self.dsem = None
        self.psum = False


class Sem:
    def __init__(self, handle, step):
        self.h = handle
        self.step = step
        self.count = 0


class Eng:
    def __init__(self, name, handle, sem):
        self.name = name
        self.h = handle
        self.sem = sem
        self.waited = {}
        self.ops = []


class _Rec:
    def __init__(self):
        self.call = None

    def __getattr__(self, name):
        def f(*a, **k):
            self.call = (name, a, k)
            return self
        return f

    def then_inc(self, *a, **k):
        return self


class Ctx:
    def __init__(self, nc, stack):
        self.nc = nc
        self.stack = stack
        self.engs = {}
        for name, h in (("pe", nc.tensor), ("act", nc.scalar), ("dve", nc.vector),
                        ("pool", nc.gpsimd), ("sp", nc.sync)):
            s = Sem(stack.enter_context(nc.semaphore("e_" + name)), 1)
            self.engs[name] = Eng(name, h, s)
        self.top = stack
        self.dsems = []
        self.free_dsems = {}
        self.nbuf = 0

    def sbuf(self, name, shape, dtype):
        self.nbuf += 1
        name = "s%d_%s" % (self.nbuf, name)
        t = self.stack.enter_context(self.nc.sbuf_tensor(name, list(shape), dtype))
        b = Buf(name)
        return t, b

    def psum(self, name, shape, dtype=F32):
        self.nbuf += 1
        name = "p%d_%s" % (self.nbuf, name)
        t = self.stack.enter_context(self.nc.psum_tensor(name, list(shape), dtype))
        b = Buf(name)
        b.psum = True
        return t, b

    def dsem_for(self, buf, kind):
        if buf.dsem is None:
            buf.dsem = {}
        if kind not in buf.dsem:
            fl = self.free_dsems.setdefault(kind, [])
            if fl:
                s = fl.pop()
            else:
                s = Sem(self.top.enter_context(self.nc.semaphore("d%s%d" % (kind, len(self.dsems)))), 16)
                self.dsems.append(s)
            buf.dsem[kind] = s
        return buf.dsem[kind]

    def _deps(self, reads, writes):
        deps = []
        for b in reads:
            if b.last_w is not None:
                deps.append(b.last_w)
            if b.psum:
                deps.extend(b.readers)
        for b in writes:
            if b.last_w is not None:
                deps.append(b.last_w)
            deps.extend(b.readers)
        return deps

    def _emit_waits(self, eng, deps, skip_sem=None):
        need = {}
        for (s, v) in deps:
            if s is skip_sem:
                continue
            if eng.waited.get(id(s), 0) >= v:
                continue
            if need.get(id(s), (None, 0))[1] < v:
                need[id(s)] = (s, v)
        for (s, v) in need.values():
            eng.waited[id(s)] = v
            eng.ops.append(("wait", s.h, v))

    def op(self, ename, fn, reads=(), writes=(), pe_chain=False):
        eng = self.engs[ename]
        deps = self._deps(reads, writes)
        self._emit_waits(eng, deps, skip_sem=eng.sem if pe_chain else None)
        eng.sem.count += 1
        tok = (eng.sem, eng.sem.count)
        rec = _Rec()
        fn(rec)
        nm, a, k = rec.call
        eng.ops.append(("op", (lambda e, nm=nm, a=a, k=k: getattr(e, nm)(*a, **k)), eng.sem.h, 1))
        for b in writes:
            b.last_w = tok
            b.readers = []
        for b in reads:
            b.readers.append(tok)
        return tok

    def dma(self, ename, out, in_, reads=(), writes=(), sembuf=None, **kw):
        eng = self.engs[ename]
        deps = self._deps(reads, writes)
        self._emit_waits(eng, deps)
        s = self.dsem_for(sembuf, 'sw' if ename == 'pool' else 'hw')
        s.count += 1
        tok = (s, s.count * 16)
        eng.ops.append(("op", (lambda e, o=out, i=in_, k=kw: e.dma_start(out=o, in_=i, **dict(dict(allow_slow_non_contiguous=True), **k))), s.h, 16))
        for b in writes:
            b.last_w = tok
            b.readers = []
        for b in reads:
            b.readers.append(tok)
        return tok

    def collective(self, kind, src_ap, dst_ap, groups, reads=(), writes=(), sembuf=None):
        eng = self.engs["pool"]
        deps = self._deps(reads, writes)
        self._emit_waits(eng, deps)
        import os
        inc = 1
        s = self.dsem_for(sembuf, "cc")
        s.count += 1
        s.step = inc
        tok = (s, s.count * inc)
        eng.ops.append(("op", (lambda e: e.collective_compute(kind, mybir.AluOpType.bypass, replica_groups=groups,
                                                               ins=[src_ap], outs=[dst_ap])), s.h, inc))
        for b in writes:
            b.last_w = tok
            b.readers = []
        for b in reads:
            b.readers.append(tok)
        return tok

    def barrier(self, bufs):
        for eng in self.engs.values():
            deps = []
            for b in bufs:
                if b.last_w is not None:
                    deps.append(b.last_w)
                deps.extend(b.readers)
            for s in self.dsems:
                if s.count > 0:
                    deps.append((s, s.count * s.step))
            for e2 in self.engs.values():
                if e2.sem.count > 0:
                    deps.append((e2.sem, e2.sem.count))
            self._emit_waits(eng, deps)
        for b in bufs:
            if b.dsem is not None:
                for kind, sm in b.dsem.items():
                    self.free_dsems.setdefault(kind, []).append(sm)
                b.dsem = None

    def finish(self, out_bufs):
        sp = self.engs["sp"]
        deps = []
        for b in out_bufs:
            if b.last_w is not None:
                deps.append(b.last_w)
        for e in self.engs.values():
            if e.sem.count > 0:
                deps.append((e.sem, e.sem.count))
        for s in self.dsems:
            if s.count > 0:
                deps.append((s, s.count * s.step))
        self._emit_waits(sp, deps)
        self.flush()

    def flush(self):
        nc = self.nc
        pend = {k: e.ops for k, e in self.engs.items()}
        for e in self.engs.values():
            e.ops = []
        with nc.Block() as block:
            def replay(e, ops):
                for o in ops:
                    if o[0] == "wait":
                        e.wait_ge(o[1], o[2])
                    else:
                        ins = o[1](e)
                        ins.then_inc(o[2], o[3])

            @block.tensor
            def _(e):
                replay(e, pend["pe"])

            @block.scalar
            def _(e):
                replay(e, pend["act"])

            @block.vector
            def _(e):
                replay(e, pend["dve"])

            @block.gpsimd
            def _(e):
                replay(e, pend["pool"])

            @block.sync
            def _(e):
                replay(e, pend["sp"])

import contextlib, math
import numpy as np

D = 2048
INC = 4416
XH = 16
TT = 512
EPS_RMS = 1e-6
EPS_LN = 1e-5
TWO_PI = 2.0 * math.pi
C1 = 6.28125
_rem = TWO_PI - C1
C2 = float(np.frombuffer((np.array([_rem], np.float32).view(np.uint32) & np.uint32(0xFFFFF000)).tobytes(), np.float32)[0])
C3 = float(np.float32(TWO_PI - C1 - C2))
MAGIC = 12582912.0
PI_CL = 3.1415925


class Ring:
    def __init__(self, c, name, shape, dtype, n, psum=False):
        self.items = []
        for i in range(n):
            self.items.append((c.psum if psum else c.sbuf)("%s%d" % (name, i), shape, dtype))
        self.i = 0

    def next(self):
        r = self.items[self.i % len(self.items)]
        self.i += 1
        return r


def consts(c):
    K = {}
    idf, idfb = c.sbuf("idf", [128, 128], F32)
    c.op("pool", lambda e: e.memset(idf[:], 1.0), writes=[idfb])
    c.op("pool", lambda e: e.affine_select(out=idf[:], in_=idf[:], pattern=[[-1, 128]], compare_op=ALU.is_equal,
                                           fill=0.0, base=0, channel_multiplier=1), reads=[idfb], writes=[idfb])
    idh, idhb = c.sbuf("idh", [128, 128], BF16)
    c.op("dve", lambda e: e.tensor_copy(out=idh[:], in_=idf[:]), reads=[idfb], writes=[idhb])
    ones, onesb = c.sbuf("ones", [128, 128], BF16)
    c.op("pool", lambda e: e.memset(ones[:], 1.0), writes=[onesb])
    ce, ceb = c.sbuf("ceps", [128, 4], F32)
    c.op("pool", lambda e: e.memset(ce[:, 0:1], EPS_RMS), writes=[ceb])
    c.op("pool", lambda e: e.memset(ce[:, 1:2], EPS_LN), writes=[ceb])
    c.op("pool", lambda e: e.memset(ce[:, 2:3], 0.0), writes=[ceb])
    K.update(idf=(idf, idfb), idh=(idh, idhb), ones=(ones, onesb), ce=(ce, ceb))
    return K


def load_cols(c, K, name, src2d, nrow):
    idf, idfb = K["idf"]
    t, tb = c.sbuf(name + "_r", [nrow, 128], F32)
    c.dma("sp", t[:], src2d, writes=[tb], sembuf=tb)
    p, pb = K["pcol"]
    c.op("pe", lambda e: e.transpose(p[:, 0:nrow], t[:], idf[0:nrow, 0:nrow]), reads=[tb, idfb], writes=[pb])
    o, ob = c.sbuf(name, [128, nrow], F32)
    c.op("dve", lambda e: e.tensor_copy(out=o[:], in_=p[:, 0:nrow]), reads=[pb], writes=[ob])
    return o, ob

def rstd_from(c, eng_out, out_ap, in_ap, scale, eps_ap, reads, wbuf):
    c.op("act", lambda e: e.activation(out=out_ap, in_=in_ap, func=AF.Ln, bias=eps_ap, scale=scale),
         reads=reads, writes=[wbuf])
    c.op("act", lambda e: e.activation(out=out_ap, in_=out_ap, func=AF.Exp, scale=-0.5),
         reads=[wbuf], writes=[wbuf])


def phase_a(c, K, NT, T):
    nc = c.nc
    NTOK = NT * TT
    ones, onesb = K["ones"]
    ce, ceb = K["ce"]
    idf, idfb = K["idf"]
    idh, idhb = K["idh"]
    xT = T["xT"]
    xTv = xT.rearrange("(c p) t -> p c t", p=128)
    w_in = T["w_in"].rearrange("(c p) n -> p c n", p=128)
    dq = Buf("dram_q"); dk = Buf("dram_k"); dv = Buf("dram_v"); dm = Buf("dram_m")

    import os
    PARTS = os.environ.get('PA_PARTS', '123')
    STOP = int(os.environ.get('PA_STOP', '99'))
    S1 = int(os.environ.get('PA_S1', '99'))
    SKIP = os.environ.get('PA_SKIP', '').split(',')
    with contextlib.ExitStack() as st:
      if '1' in PARTS:
          c.stack, old = st, c.stack
          wq, wqb = c.sbuf("wq", [128, 16, 768], BF16)
          wc, wcb = c.sbuf("wc", [128, 16, 576], BF16)
          wqr, wqrb = c.sbuf("wqr", [128, 16, 4, 64], BF16)
          wkr, wkrb = c.sbuf("wkr", [128, 16, 64], BF16)
          wuf, wufb = c.sbuf("wuf", [128, 4, 1024], F32)
          wu, wub = c.sbuf("wu", [128, 4, 1024], BF16)
          invf, invfb = c.sbuf("invf", [64, 1], F32)
          for k0 in range(0, 16, 4):
              c.dma("pool", wq[:, k0:k0 + 4, :], w_in[:, k0:k0 + 4, 0:768], writes=[wqb], sembuf=wqb)
              c.dma("pool", wc[:, k0:k0 + 4, :], w_in[:, k0:k0 + 4, 768:1344], writes=[wcb], sembuf=wcb)
          c.dma("sp", wuf[:], T["w_ukv"].rearrange("(c p) n -> p c n", p=128), writes=[wufb], sembuf=wufb)
          K["pcol"] = c.psum("pcol", [128, 16], F32)
          kg, kgb = load_cols(c, K, "kg", T["kvg"].rearrange("(c p) -> c p", p=128), 4)
          c.dma("sp", invf[:], T["invf"].rearrange("(p o) -> p o", o=1), writes=[invfb], sembuf=invfb)
          for k in range(4):
              c.op("dve", lambda e, k=k: e.tensor_scalar(out=wu[:, k, :], in0=wuf[:, k, :], scalar1=kg[:, k:k + 1],
                                                         scalar2=None, op0=ALU.mult), reads=[wufb, kgb], writes=[wub])
          wq4 = wq[:].rearrange("p c (h d) -> p c h d", h=4)
          for k in range(16):
              c.op("dve", lambda e, k=k: e.tensor_scalar(out=wqr[:, k, :, 0:32], in0=wq4[:, k, :, 160:192], scalar1=-1.0,
                                                         scalar2=None, op0=ALU.mult), reads=[wqb], writes=[wqrb])
              c.op("pool", lambda e, k=k: e.tensor_copy(out=wqr[:, k, :, 32:64], in_=wq4[:, k, :, 128:160]),
                   reads=[wqb], writes=[wqrb])
          c.op("dve", lambda e: e.tensor_scalar(out=wkr[:, :, 0:32], in0=wc[:, :, 544:576], scalar1=-1.0, scalar2=None,
                                                op0=ALU.mult), reads=[wcb], writes=[wkrb])
          c.op("pool", lambda e: e.tensor_copy(out=wkr[:, :, 32:64], in_=wc[:, :, 512:544]), reads=[wcb], writes=[wkrb])

          xr = Ring(c, "xt", [128, 16, TT], BF16, 2)
          posi, posib = c.sbuf("posi", [64, TT], I32)
          ang, angb = c.sbuf("ang", [64, TT], F32)
          rn, rnb = c.sbuf("rn", [64, TT], F32)
          rr, rrb = c.sbuf("rr", [64, TT], F32)
          sn, snb = c.sbuf("sin", [64, TT], F32)
          cs, csb = c.sbuf("cos", [64, TT], F32)
          t1r = Ring(c, "t1", [64, TT], F32, 2)
          t2r = Ring(c, "t2", [64, TT], F32, 2)
          obr = Ring(c, "ob", [128, TT], BF16, 4)
          ovr = Ring(c, "ov", [128, 512], BF16, 2)
          sq, sqb = c.sbuf("sq", [128, 4, TT], BF16)
          ck, ckb = c.sbuf("ck", [128, 4, TT], BF16)
          rsF, rsFb = c.sbuf("rsF", [128, TT], F32)
          rsT, rsTb = c.sbuf("rsT", [128, 4], F32)
          pr = Ring(c, "pA", [128, TT], F32, 4, psum=True)
          pr2 = Ring(c, "pB", [128, TT], F32, 1, psum=True)
          pss, pssb = c.psum("pss", [128, TT], F32)
          pst, pstb = c.psum("pst", [128, 4], F32)

          for i in range(NT):
              col0 = XH + i * TT
              xt, xtb = xr.next()
              c.dma("sp", xt[:], xTv[:, :, col0:col0 + TT], writes=[xtb], sembuf=xtb)
              c.dma("sp", posi[:], T["pos"][i * TT:(i + 1) * TT].partition_broadcast(64), writes=[posib], sembuf=posib)
              c.op("dve", lambda e: e.tensor_copy(out=ang[:], in_=posi[:]), reads=[posib], writes=[angb])
              c.op("dve", lambda e: e.tensor_scalar(out=ang[:], in0=ang[:], scalar1=invf[:, 0:1], scalar2=None, op0=ALU.mult),
                   reads=[angb, invfb], writes=[angb])
              c.op("dve", lambda e: e.tensor_scalar(out=rn[:], in0=ang[:], scalar1=1.0 / TWO_PI, scalar2=MAGIC,
                                                    op0=ALU.mult, op1=ALU.add), reads=[angb], writes=[rnb])
              c.op("dve", lambda e: e.tensor_scalar(out=rn[:], in0=rn[:], scalar1=-MAGIC, scalar2=None, op0=ALU.add),
                   reads=[rnb], writes=[rnb])
              c.op("dve", lambda e: e.scalar_tensor_tensor(out=rr[:], in0=rn[:], scalar=-C1, in1=ang[:], op0=ALU.mult, op1=ALU.add),
                   reads=[rnb, angb], writes=[rrb])
              c.op("dve", lambda e: e.scalar_tensor_tensor(out=rr[:], in0=rn[:], scalar=-C2, in1=rr[:], op0=ALU.mult, op1=ALU.add),
                   reads=[rnb, rrb], writes=[rrb])
              c.op("dve", lambda e: e.scalar_tensor_tensor(out=rr[:], in0=rn[:], scalar=-C3, in1=rr[:], op0=ALU.mult, op1=ALU.add),
                   reads=[rnb, rrb], writes=[rrb])
              c.op("dve", lambda e: e.tensor_scalar(out=ang[:], in0=rr[:], scalar1=math.pi / 2, scalar2=-TWO_PI,
                                                    op0=ALU.is_gt, op1=ALU.mult), reads=[rrb], writes=[angb])
              c.op("dve", lambda e: e.scalar_tensor_tensor(out=ang[:], in0=rr[:], scalar=math.pi / 2, in1=ang[:],
                                                           op0=ALU.add, op1=ALU.add), reads=[rrb, angb], writes=[angb])
              c.op("dve", lambda e: e.tensor_scalar(out=rr[:], in0=rr[:], scalar1=PI_CL, scalar2=-PI_CL, op0=ALU.min, op1=ALU.max),
                   reads=[rrb], writes=[rrb])
              c.op("dve", lambda e: e.tensor_scalar(out=ang[:], in0=ang[:], scalar1=PI_CL, scalar2=-PI_CL, op0=ALU.min, op1=ALU.max),
                   reads=[angb], writes=[angb])
              c.op("act", lambda e: e.activation(out=sn[:], in_=rr[:], func=AF.Sin), reads=[rrb], writes=[snb])
              c.op("act", lambda e: e.activation(out=cs[:], in_=ang[:], func=AF.Sin), reads=[angb], writes=[csb])

              def rope_block(wa, wrot, dst_ap, dbuf):
                  pa, pab = pr.next()
                  pb, pbb = pr.next()
                  for k in range(16):
                      c.op("pe", lambda e, k=k: e.matmul(pa[0:64, :], wa(k), xt[:, k, :], start=(k == 0), stop=(k == 15)),
                           reads=[xtb, wqb, wcb], writes=[pab], pe_chain=(k > 0))
                  for k in range(16):
                      c.op("pe", lambda e, k=k: e.matmul(pb[0:64, :], wrot(k), xt[:, k, :], start=(k == 0), stop=(k == 15)),
                           reads=[xtb, wqrb, wkrb], writes=[pbb], pe_chain=(k > 0))
                  t1, t1b = t1r.next()
                  t2, t2b = t2r.next()
                  c.op("dve", lambda e: e.tensor_tensor(out=t1[:], in0=pa[0:64, :], in1=cs[:], op=ALU.mult), reads=[pab, csb], writes=[t1b])
                  c.op("dve", lambda e: e.tensor_tensor(out=t2[:], in0=pb[0:64, :], in1=sn[:], op=ALU.mult), reads=[pbb, snb], writes=[t2b])
                  ob, obb = obr.next()
                  c.op("dve", lambda e: e.tensor_tensor(out=ob[0:64, :], in0=t1[:], in1=t2[:], op=ALU.add), reads=[t1b, t2b], writes=[obb])
                  c.dma("sp", dst_ap, ob[0:64, :], reads=[obb], writes=[dbuf], sembuf=obb)

              for h in range(4 if S1 >= 3 else 0):
                  pa, pab = pr.next()
                  for k in range(16):
                      c.op("pe", lambda e, k=k, h=h: e.matmul(pa[:], wq[:, k, 192 * h:192 * h + 128], xt[:, k, :], start=(k == 0), stop=(k == 15)),
                           reads=[xtb, wqb], writes=[pab], pe_chain=(k > 0))
                  ob, obb = obr.next()
                  c.op("act", lambda e: e.activation(out=ob[:], in_=pa[:], func=AF.Copy), reads=[pab], writes=[obb])
                  c.dma("sp", T["QT"][h, 0:128, i * TT:(i + 1) * TT], ob[:], reads=[obb], writes=[dq], sembuf=obb)
                  rope_block(lambda k, h=h: wq[:, k, 192 * h + 128:192 * h + 192], lambda k, h=h: wqr[:, k, h, :],
                             T["QT"][h, 128:192, i * TT:(i + 1) * TT], dq)
              S1 >= 4 and 'kr' not in SKIP and rope_block(lambda k: wc[:, k, 512:576], lambda k: wkr[:, k, :], T["KTt"](i)[512:576, :], dk)
              for ch in range(4 if S1 >= 4 and 'ckv' not in SKIP else 0):
                  pa, pab = pr.next()
                  for k in range(16):
                      c.op("pe", lambda e, k=k, ch=ch: e.matmul(pa[:], wc[:, k, 128 * ch:128 * ch + 128], xt[:, k, :], start=(k == 0), stop=(k == 15)),
                           reads=[xtb, wcb], writes=[pab], pe_chain=(k > 0))
                  c.op("act", lambda e, ch=ch: e.activation(out=sq[:, ch, :], in_=pa[:], func=AF.Square), reads=[pab], writes=[sqb])
                  c.op("act", lambda e, ch=ch: e.activation(out=ck[:, ch, :], in_=pa[:], func=AF.Copy), reads=[pab], writes=[ckb])
              for ch in range(4 if S1 >= 5 else 0):
                  c.op("pe", lambda e, ch=ch: e.matmul(pss[:], ones[:], sq[:, ch, :], start=(ch == 0), stop=(ch == 3)),
                       reads=[sqb, onesb], writes=[pssb], pe_chain=(ch > 0))
              for sub in range(4 if S1 >= 6 else 0):
                  for ch in range(4):
                      c.op("pe", lambda e, ch=ch, sub=sub: e.matmul(pst[:, sub:sub + 1], sq[:, ch, sub * 128:(sub + 1) * 128], ones[:, 0:1],
                                                                   start=(ch == 0), stop=(ch == 3)),
                           reads=[sqb, onesb], writes=[pstb], pe_chain=(ch > 0 or sub > 0))
              S1 >= 5 and rstd_from(c, "act", rsF[:], pss[:], 1.0 / 512, ce[:, 0:1], [pssb, ceb], rsFb)
              S1 >= 6 and rstd_from(c, "act", rsT[:], pst[:], 1.0 / 512, ce[:, 0:1], [pstb, ceb], rsTb)
              for h in range(4 if S1 >= 7 else 0):
                  pa, pab = pr.next()
                  for k in range(4):
                      c.op("pe", lambda e, k=k, h=h: e.matmul(pa[:], wu[:, k, 256 * h:256 * h + 128], ck[:, k, :], start=(k == 0), stop=(k == 3)),
                           reads=[ckb, wub], writes=[pab], pe_chain=(k > 0))
                  ob, obb = obr.next()
                  c.op("dve", lambda e: e.tensor_tensor(out=ob[:], in0=pa[:], in1=rsF[:], op=ALU.mult), reads=[pab, rsFb], writes=[obb])
                  c.dma("sp", T["KTt"](i)[128 * h:128 * h + 128, :], ob[:], reads=[obb], writes=[dk], sembuf=obb)
              wuv = wu[:].rearrange("p c (h d) -> p c h d", h=4)
              for sub in range(4 if S1 >= 8 else 0):
                  pa, pab = pr2.next()
                  pav = pa[:].rearrange("p (h d) -> p h d", h=4)
                  for k in range(4):
                      c.op("pe", lambda e, k=k, sub=sub: e.matmul(pav, ck[:, k, sub * 128:(sub + 1) * 128], wuv[:, k, :, 128:256], start=(k == 0), stop=(k == 3)),
                           reads=[ckb, wub], writes=[pab], pe_chain=(k > 0))
                  ov, ovb = ovr.next()
                  c.op("dve", lambda e, sub=sub: e.tensor_scalar(out=ov[:], in0=pa[:], scalar1=rsT[:, sub:sub + 1], scalar2=None, op0=ALU.mult),
                       reads=[pab, rsTb], writes=[ovb])
                  t0 = i * TT + sub * 128
                  c.dma("sp", T["V"][t0:t0 + 128, :], ov[:], reads=[ovb], writes=[dv], sembuf=ovb)
          c.barrier([b for (_, b) in xr.items + obr.items + ovr.items + pr.items + pr2.items] + [wqb, wcb, wqrb, wkrb, wub, wufb, sqb, ckb, rsFb, rsTb, pssb, pstb, snb, csb, angb, rrb, rnb, posib, kgb, invfb] + [b for (_, b) in t1r.items + t2r.items])
          c.flush()
          c.stack = old

    with contextlib.ExitStack() as st:
      if '2' in PARTS:
          c.stack, old = st, c.stack
          w3, w3b = c.sbuf("w3", [128, 16, 1536], BF16)
          for k0 in range(0, 16, 2):
              c.dma("pool", w3[:, k0:k0 + 2, :], w_in[:, k0:k0 + 2, 1344:2880], writes=[w3b], sembuf=w3b)
          cw, cwb = c.sbuf("cw", [128, 3, 4], F32)
          c.dma("sp", cw[:], T["conv_w"].rearrange("j (c p) -> p j c", p=128), writes=[cwb], sembuf=cwb)
          gn, gnb = c.sbuf("gain", [128, 16], F32)
          c.dma("sp", gn[:], T["gain"].rearrange("(c p) -> p c", p=128), writes=[gnb], sembuf=gnb)
          xr = Ring(c, "xt", [128, 16, TT], BF16, 2)
          xh, xhb = c.sbuf("xh", [128, 16, XH], BF16)
          gbuf, gbb = c.sbuf("gbuf", [128, 4, 2 + TT], F32)
          hs, hsb = c.sbuf("hs", [128, TT], F32)
          acc, accb = c.sbuf("acc", [128, TT], F32)
          yc, ycb = c.sbuf("yc", [128, 4, TT], F32)
          sq, sqb = c.sbuf("sq", [128, 4, TT], BF16)
          rsF, rsFb = c.sbuf("rsF", [128, TT], F32)
          obr = Ring(c, "ob", [128, TT], BF16, 4)
          pr = Ring(c, "pA", [128, TT], F32, 6, psum=True)
          pss, pssb = c.psum("pss", [128, TT], F32)
          c.dma("sp", xh[:], xTv[:, :, 0:XH], writes=[xhb], sembuf=xhb)
          for ch in range(4 if STOP >= 1 else 0):
              pc, pcb = pr.next()
              ph, phb = pr.next()
              for k in range(16):
                  c.op("pe", lambda e, k=k, ch=ch: e.matmul(pc[:, 0:XH], w3[:, k, 512 + 128 * ch:512 + 128 * ch + 128], xh[:, k, :], start=(k == 0), stop=(k == 15)),
                       reads=[xhb, w3b], writes=[pcb], pe_chain=(k > 0))
              for k in range(16):
                  c.op("pe", lambda e, k=k, ch=ch: e.matmul(ph[:, 0:XH], w3[:, k, 1024 + 128 * ch:1024 + 128 * ch + 128], xh[:, k, :], start=(k == 0), stop=(k == 15)),
                       reads=[xhb, w3b], writes=[phb], pe_chain=(k > 0))
              c.op("act", lambda e: e.activation(out=hs[:, 0:XH], in_=ph[:, 0:XH], func=AF.Copy), reads=[phb], writes=[hsb])
              c.op("dve", lambda e, ch=ch: e.tensor_tensor(out=gbuf[:, ch, 0:2], in0=pc[:, XH - 2:XH], in1=hs[:, XH - 2:XH], op=ALU.mult),
                   reads=[pcb, hsb], writes=[gbb])
          for i in range(NT if STOP >= 2 else 0):
              col0 = XH + i * TT
              xt, xtb = xr.next()
              c.dma("sp", xt[:], xTv[:, :, col0:col0 + TT], writes=[xtb], sembuf=xtb)
              for ch in range(4):
                  pb_, pbb = pr.next()
                  pc, pcb = pr.next()
                  ph, phb = pr.next()
                  for (pp, ppb, off) in ((pb_, pbb, 0), (pc, pcb, 512), (ph, phb, 1024)):
                      for k in range(16):
                          c.op("pe", lambda e, k=k, pp=pp, off=off, ch=ch: e.matmul(pp[:], w3[:, k, off + 128 * ch:off + 128 * ch + 128], xt[:, k, :], start=(k == 0), stop=(k == 15)),
                               reads=[xtb, w3b], writes=[ppb], pe_chain=(k > 0))
                  c.op("act", lambda e: e.activation(out=hs[:], in_=ph[:], func=AF.Copy), reads=[phb], writes=[hsb])
                  c.op("dve", lambda e, ch=ch: e.tensor_tensor(out=gbuf[:, ch, 2:2 + TT], in0=pc[:], in1=hs[:], op=ALU.mult),
                       reads=[pcb, hsb], writes=[gbb])
                  c.op("dve", lambda e, ch=ch: e.tensor_scalar(out=acc[:], in0=gbuf[:, ch, 0:TT], scalar1=cw[:, 0, ch:ch + 1], scalar2=None, op0=ALU.mult),
                       reads=[gbb, cwb], writes=[accb])
                  c.op("dve", lambda e, ch=ch: e.scalar_tensor_tensor(out=acc[:], in0=gbuf[:, ch, 1:1 + TT], scalar=cw[:, 1, ch:ch + 1], in1=acc[:], op0=ALU.mult, op1=ALU.add),
                       reads=[gbb, cwb, accb], writes=[accb])
                  c.op("dve", lambda e, ch=ch: e.scalar_tensor_tensor(out=acc[:], in0=gbuf[:, ch, 2:2 + TT], scalar=cw[:, 2, ch:ch + 1], in1=acc[:], op0=ALU.mult, op1=ALU.add),
                       reads=[gbb, cwb, accb], writes=[accb])
                  c.op("dve", lambda e, ch=ch: e.tensor_tensor(out=yc[:, ch, :], in0=pb_[:], in1=acc[:], op=ALU.mult), reads=[pbb, accb], writes=[ycb])
                  c.op("act", lambda e, ch=ch: e.activation(out=sq[:, ch, :], in_=yc[:, ch, :], func=AF.Square), reads=[ycb], writes=[sqb])
                  c.op("pool", lambda e, ch=ch: e.tensor_copy(out=gbuf[:, ch, 0:2], in_=gbuf[:, ch, TT:TT + 2]), reads=[gbb], writes=[gbb])
              for ch in range(4 if STOP >= 3 else 0):
                  c.op("pe", lambda e, ch=ch: e.matmul(pss[:], ones[:], sq[:, ch, :], start=(ch == 0), stop=(ch == 3)),
                       reads=[sqb, onesb], writes=[pssb], pe_chain=(ch > 0))
              STOP >= 4 and rstd_from(c, "act", rsF[:], pss[:], 1.0 / 512, ce[:, 0:1], [pssb, ceb], rsFb)
              for ch in range(4 if STOP >= 5 else 0):
                  ob, obb = obr.next()
                  c.op("dve", lambda e, ch=ch: e.scalar_tensor_tensor(out=ob[:], in0=yc[:, ch, :], scalar=gn[:, 4 + ch:5 + ch], in1=rsF[:], op0=ALU.mult, op1=ALU.mult),
                       reads=[ycb, gnb, rsFb], writes=[obb])
                  if STOP >= 6:
                      c.dma("sp", T["mT"][128 * ch:128 * ch + 128, i * TT:(i + 1) * TT], ob[:], reads=[obb], writes=[dm], sembuf=obb)
          c.barrier([b for (_, b) in xr.items + obr.items + pr.items] + [w3b, cwb, gnb, xhb, gbb, hsb, accb, ycb, sqb, rsFb, pssb])
          c.flush()
          c.stack = old

    with contextlib.ExitStack() as st:
      if '3' in PARTS:
          c.stack, old = st, c.stack
          w3, w3b = c.sbuf("w3", [128, 16, 1536], BF16)
          for k0 in range(0, 16, 2):
              c.dma("pool", w3[:, k0:k0 + 2, :], w_in[:, k0:k0 + 2, 2880:4416], writes=[w3b], sembuf=w3b)
          gn, gnb = c.sbuf("gain", [128, 16], F32)
          c.dma("sp", gn[:], T["gain"].rearrange("(c p) -> p c", p=128), writes=[gnb], sembuf=gnb)
          gnbc, gnbcb = c.sbuf("gnbc", [128, 512], F32)
          c.dma("sp", gnbc[:], T["gain"][1536:2048].partition_broadcast(128), writes=[gnbcb], sembuf=gnbcb)
          lg, lgb = c.sbuf("lng", [128, 512], F32)
          lb, lbb = c.sbuf("lnb", [128, 512], F32)
          c.dma("sp", lg[:], T["sgu_ln_g"].partition_broadcast(128), writes=[lgb], sembuf=lgb)
          c.dma("sp", lb[:], T["sgu_ln_b"].partition_broadcast(128), writes=[lbb], sembuf=lbb)
          sb_, sbb = c.sbuf("sgub", [128, 4], F32)
          c.dma("sp", sb_[:], T["sgu_b"].rearrange("g t -> t g"), writes=[sbb], sembuf=sbb, allow_slow_non_contiguous=True)
          pw, pwb = c.sbuf("poolw", [128, 4, 128], BF16)
          c.dma("pool", pw[:], T["pool_w"].rearrange("g c d -> c g d"), writes=[pwb], sembuf=pwb)
          invc, invcb = c.sbuf("invc", [128, 4, XH], F32)
          c.dma("sp", invc[:], T["invc"].partition_broadcast(128), writes=[invcb], sembuf=invcb)
          swf, swfb = c.sbuf("swf", [128, 4, 128], F32)
          c.dma("sp", swf[:], T["sgu_w"].rearrange("g t s -> t g s"), writes=[swfb], sembuf=swfb)
          wsT, wsTb = c.sbuf("wsT", [128, 4, 128], BF16)
          pr = Ring(c, "pA", [128, TT], F32, 4, psum=True)
          pm_r = Ring(c, "pM", [128, TT], F32, 1, psum=True)
          ptr, ptrb = c.psum("ptr", [128, 4, 128], BF16)
          pss, pssb = c.psum("pss", [128, TT], F32)
          for g in range(4):
              c.op("pool", lambda e, g=g: e.affine_select(out=swf[:, g, :], in_=swf[:, g, :], pattern=[[-1, 128]], compare_op=ALU.is_ge,
                                                         fill=0.0, base=0, channel_multiplier=1), reads=[swfb], writes=[swfb])
              pa, pab = pr.next()
              c.op("pe", lambda e, g=g: e.transpose(pa[:, 0:128], swf[:, g, :], idf[:]), reads=[swfb, idfb], writes=[pab])
              c.op("act", lambda e, g=g: e.activation(out=wsT[:, g, :], in_=pa[:, 0:128], func=AF.Copy), reads=[pab], writes=[wsTb])
          xr = Ring(c, "xt", [128, 16, TT], BF16, 2)
          xh, xhb = c.sbuf("xh", [128, 16, XH], BF16)
          hbuf, hbb = c.sbuf("hbuf", [128, 4, XH + TT], F32)
          sa, sab = c.sbuf("sa", [128, XH + TT], F32)
          sb2, sb2b = c.sbuf("sb2", [128, XH + TT], F32)
          pbf, pbfb = c.sbuf("pbf", [128, TT], BF16)
          yp, ypb = c.sbuf("yp", [128, 4, TT], F32)
          sq, sqb = c.sbuf("sq", [128, 4, TT], BF16)
          rsF, rsFb = c.sbuf("rsF", [128, TT], F32)
          obr = Ring(c, "ob", [128, TT], BF16, 4)
          gu, gub = c.sbuf("gu", [128, 512], F32)
          gv, gvb = c.sbuf("gv", [128, 512], F32)
          vnb, vnbb = c.sbuf("vnb", [128, 512], BF16)
          ys, ysb = c.sbuf("ys", [128, 512], F32)
          junk, junkb = c.sbuf("junk", [128, 512], BF16)
          ym, ymb = c.sbuf("ym", [128, 512], BF16)
          msg, msgb = c.sbuf("msg", [128, 4, TT], BF16)
          bn, bnb = c.sbuf("bn", [128, 6], F32)
          sts, stsb = c.sbuf("sts", [128, 8], F32)
          c.dma("sp", xh[:], xTv[:, :, 0:XH], writes=[xhb], sembuf=xhb)
          for g in range(4):
              ph, phb = pr.next()
              for k in range(16):
                  c.op("pe", lambda e, k=k, g=g: e.matmul(ph[:, 0:XH], w3[:, k, 128 * g:128 * g + 128], xh[:, k, :], start=(k == 0), stop=(k == 15)),
                       reads=[xhb, w3b], writes=[phb], pe_chain=(k > 0))
              c.op("act", lambda e, g=g: e.activation(out=hbuf[:, g, 0:XH], in_=ph[:, 0:XH], func=AF.Copy), reads=[phb], writes=[hbb])
          NB = XH + TT
          for i in range(NT):
              col0 = XH + i * TT
              xt, xtb = xr.next()
              c.dma("sp", xt[:], xTv[:, :, col0:col0 + TT], writes=[xtb], sembuf=xtb)
              for g in range(4):
                  ph, phb = pr.next()
                  for k in range(16):
                      c.op("pe", lambda e, k=k, g=g: e.matmul(ph[:], w3[:, k, 128 * g:128 * g + 128], xt[:, k, :], start=(k == 0), stop=(k == 15)),
                           reads=[xtb, w3b], writes=[phb], pe_chain=(k > 0))
                  c.op("act", lambda e, g=g: e.activation(out=hbuf[:, g, XH:NB], in_=ph[:], func=AF.Copy), reads=[phb], writes=[hbb])
                  hb = hbuf[:, g, :]
                  c.op("dve", lambda e, hb=hb: e.tensor_tensor(out=sa[:, 1:NB], in0=hb[:, 1:NB], in1=hb[:, 0:NB - 1], op=ALU.add), reads=[hbb], writes=[sab])
                  S, Sb = sa, sab
                  if g >= 1:
                      c.op("dve", lambda e: e.tensor_tensor(out=sb2[:, 3:NB], in0=sa[:, 3:NB], in1=sa[:, 1:NB - 2], op=ALU.add), reads=[sab], writes=[sb2b])
                      S, Sb = sb2, sb2b
                  if g >= 2:
                      c.op("dve", lambda e: e.tensor_tensor(out=sa[:, 7:NB], in0=sb2[:, 7:NB], in1=sb2[:, 3:NB - 4], op=ALU.add), reads=[sb2b], writes=[sab])
                      S, Sb = sa, sab
                  if g >= 3:
                      c.op("dve", lambda e: e.tensor_tensor(out=sb2[:, 15:NB], in0=sa[:, 15:NB], in1=sa[:, 7:NB - 8], op=ALU.add), reads=[sab], writes=[sb2b])
                      S, Sb = sb2, sb2b
                  wgt = 1.0 / (2 << g)
                  if i == 0:
                      c.op("dve", lambda e, S=S, g=g: e.tensor_tensor(out=S[:, XH:2 * XH], in0=S[:, XH:2 * XH], in1=invc[:, g, :], op=ALU.mult),
                           reads=[Sb, invcb], writes=[Sb])
                      c.op("dve", lambda e, S=S, hb=hb: e.tensor_tensor(out=pbf[:, 0:XH], in0=S[:, XH:2 * XH], in1=hb[:, XH:2 * XH], op=ALU.subtract),
                           reads=[Sb, hbb], writes=[pbfb])
                      c.op("dve", lambda e, S=S, hb=hb, wgt=wgt: e.scalar_tensor_tensor(out=pbf[:, XH:TT], in0=S[:, 2 * XH:NB], scalar=wgt, in1=hb[:, 2 * XH:NB], op0=ALU.mult, op1=ALU.subtract),
                           reads=[Sb, hbb], writes=[pbfb])
                  else:
                      c.op("dve", lambda e, S=S, hb=hb, wgt=wgt: e.scalar_tensor_tensor(out=pbf[:], in0=S[:, XH:NB], scalar=wgt, in1=hb[:, XH:NB], op0=ALU.mult, op1=ALU.subtract),
                           reads=[Sb, hbb], writes=[pbfb])
                  py, pyb = pr.next()
                  c.op("pe", lambda e, g=g: e.matmul(py[:], pw[:, g, :], pbf[:], start=True, stop=True), reads=[pwb, pbfb], writes=[pyb])
                  c.op("act", lambda e, g=g: e.activation(out=yp[:, g, :], in_=py[:], func=AF.Copy), reads=[pyb], writes=[ypb])
                  c.op("act", lambda e, g=g: e.activation(out=sq[:, g, :], in_=py[:], func=AF.Square), reads=[pyb], writes=[sqb])
                  c.op("pool", lambda e, g=g: e.tensor_copy(out=hbuf[:, g, 0:XH], in_=hbuf[:, g, TT:NB]), reads=[hbb], writes=[hbb])
              for ch in range(4):
                  c.op("pe", lambda e, ch=ch: e.matmul(pss[:], ones[:], sq[:, ch, :], start=(ch == 0), stop=(ch == 3)),
                       reads=[sqb, onesb], writes=[pssb], pe_chain=(ch > 0))
              rstd_from(c, "act", rsF[:], pss[:], 1.0 / 512, ce[:, 0:1], [pssb, ceb], rsFb)
              for ch in range(4):
                  ob, obb = obr.next()
                  c.op("dve", lambda e, ch=ch: e.scalar_tensor_tensor(out=ob[:], in0=yp[:, ch, :], scalar=gn[:, 8 + ch:9 + ch], in1=rsF[:], op0=ALU.mult, op1=ALU.mult),
                       reads=[ypb, gnb, rsFb], writes=[obb])
                  c.dma("sp", T["mT"][512 + 128 * ch:512 + 128 * ch + 128, i * TT:(i + 1) * TT], ob[:], reads=[obb], writes=[dm], sembuf=obb)
              for sub in range(4):
                  pu, pub = pr.next()
                  pv, pvb = pr.next()
                  for (pp, ppb, off) in ((pu, pub, 512), (pv, pvb, 1024)):
                      for k in range(16):
                          c.op("pe", lambda e, k=k, pp=pp, off=off, sub=sub: e.matmul(pp[:], xt[:, k, sub * 128:(sub + 1) * 128], w3[:, k, off:off + 512], start=(k == 0), stop=(k == 15)),
                               reads=[xtb, w3b], writes=[ppb], pe_chain=(k > 0))
                  c.op("act", lambda e: e.activation(out=gu[:], in_=pu[:], func=AF.Gelu), reads=[pub], writes=[gub])
                  c.op("act", lambda e: e.activation(out=gv[:], in_=pv[:], func=AF.Gelu), reads=[pvb], writes=[gvb])
                  c.op("dve", lambda e: e.bn_stats(out=bn[:], in_=gv[:]), reads=[gvb], writes=[bnb])
                  c.op("dve", lambda e: e.bn_aggr(out=sts[:, 0:2], in_=bn[:]), reads=[bnb], writes=[stsb])
                  rstd_from(c, "act", sts[:, 2:3], sts[:, 1:2], 1.0, ce[:, 1:2], [stsb, ceb], stsb)
                  c.op("dve", lambda e: e.tensor_scalar(out=gv[:], in0=gv[:], scalar1=sts[:, 0:1], scalar2=sts[:, 2:3], op0=ALU.subtract, op1=ALU.mult),
                       reads=[gvb, stsb], writes=[gvb])
                  c.op("dve", lambda e: e.tensor_tensor(out=gv[:], in0=gv[:], in1=lg[:], op=ALU.mult), reads=[gvb, lgb], writes=[gvb])
                  c.op("dve", lambda e: e.tensor_tensor(out=vnb[:], in0=gv[:], in1=lb[:], op=ALU.add), reads=[gvb, lbb], writes=[vnbb])
                  pm, pmb = pm_r.next()
                  for g in range(4):
                      c.op("pe", lambda e, g=g: e.matmul(pm[:, 128 * g:128 * g + 128], wsT[:, g, :], vnb[:, 128 * g:128 * g + 128], start=True, stop=True),
                           reads=[wsTb, vnbb], writes=[pmb], pe_chain=(g > 0))
                  for g in range(4):
                      c.op("dve", lambda e, g=g: e.scalar_tensor_tensor(out=ys[:, 128 * g:128 * g + 128], in0=pm[:, 128 * g:128 * g + 128], scalar=sb_[:, g:g + 1],
                                                                       in1=gu[:, 128 * g:128 * g + 128], op0=ALU.add, op1=ALU.mult),
                           reads=[pmb, sbb, gub], writes=[ysb])
                  c.op("act", lambda e: e.activation(out=junk[:], in_=ys[:], func=AF.Square, accum_out=sts[:, 4:5]), reads=[ysb], writes=[junkb, stsb])
                  rstd_from(c, "act", sts[:, 5:6], sts[:, 4:5], 1.0 / 512, ce[:, 0:1], [stsb, ceb], stsb)
                  c.op("dve", lambda e: e.scalar_tensor_tensor(out=ym[:], in0=ys[:], scalar=sts[:, 5:6], in1=gnbc[:], op0=ALU.mult, op1=ALU.mult),
                       reads=[ysb, stsb, gnbcb], writes=[ymb])
                  for k in range(4):
                      c.op("pe", lambda e, k=k: e.transpose(ptr[:, k, :], ym[:, 128 * k:128 * k + 128], idh[:]), reads=[ymb, idhb], writes=[ptrb], pe_chain=(k > 0))
                  c.op("act", lambda e, sub=sub: e.activation(out=msg[:, :, sub * 128:(sub + 1) * 128], in_=ptr[:], func=AF.Copy), reads=[ptrb], writes=[msgb])
              c.dma("sp", T["mT"][1024:1536, i * TT:(i + 1) * TT].rearrange("(c p) t -> p c t", p=128), msg[:], reads=[msgb], writes=[dm], sembuf=msgb)
          c.barrier([b for (_, b) in xr.items + obr.items + pr.items + pm_r.items] + [w3b, gnb, gnbcb, lgb, lbb, sbb, pwb, invcb, swfb, wsTb, ptrb, pssb, xhb, hbb, sab, sb2b, pbfb, ypb, sqb, rsFb, gub, gvb, vnbb, ysb, junkb, ymb, msgb, bnb, stsb])
          c.flush()
          c.stack = old
    return [dq, dk, dv, dm]

import contextlib, math
import numpy as np

ALPHA = (2.0 * 4) ** 0.25
ATT_SCALE = 1.0 / math.sqrt(192.0)
DFF = 5632
NF = DFF // 128


def phase_b1a(c, K, NT, NPAST, T):
    ones, onesb = K["ones"]
    ce, ceb = K["ce"]
    dm0 = Buf("dram_m0")
    with contextlib.ExitStack() as st:
        c.stack, old = st, c.stack
        K["pcol"] = c.psum("pcol", [128, 16], F32)
        gn, gnb = load_cols(c, K, "gain", T["gain"].rearrange("(c p) -> c p", p=128), 16)
        qb, qbb = c.sbuf("qb", [128, 4], F32)
        c.dma("sp", qb[:, 0:max(NPAST, 1)], T["qbias"], writes=[qbb], sembuf=qbb)
        qnr = Ring(c, "qn", [128, 2, TT], BF16, 2)
        qrr = Ring(c, "qr", [64, 2, TT], BF16, 2)
        knr = Ring(c, "kn", [128, 2, 512], BF16, 3)
        krr = Ring(c, "kr", [64, 512], BF16, 3)
        vvr = Ring(c, "vv", [128, 4, 256], BF16, 3)
        ptr_ = Ring(c, "pT", [128, TT], BF16, 4)
        ya, yab = c.sbuf("ya", [128, 4, TT], F32)
        sq, sqb = c.sbuf("sq", [128, 4, TT], BF16)
        rl, rlb = c.sbuf("rl", [128, TT], F32)
        rsF, rsFb = c.sbuf("rsF", [128, TT], F32)
        obr = Ring(c, "ob", [128, TT], BF16, 4)
        Sr = Ring(c, "pS", [128, TT], F32, 3, psum=True)
        Or = [c.psum("pO%d" % k, [128, TT], F32) for k in range(2)]
        Lr = [c.psum("pL%d" % k, [128, TT], F32) for k in range(2)]
        for i in range(NT):
            cs = slice(i * TT, (i + 1) * TT)
            for hp in range(2):
                qn, qnb = qnr.next()
                qr, qrb = qrr.next()
                c.dma("sp", qn[:], T["QT"][2 * hp:2 * hp + 2, 0:128, cs].rearrange("h d t -> d h t"), writes=[qnb], sembuf=qnb)
                c.dma("sp", qr[:], T["QT"][2 * hp:2 * hp + 2, 128:192, cs].rearrange("h d t -> d h t"), writes=[qrb], sembuf=qrb)
                chunks = [(j, cc) for j in range(NPAST) for cc in range(NT)] + [(None, cc) for cc in range(i + 1)]
                nblk = len(chunks) * 4
                bi = 0
                for (j, cc) in chunks:
                    kc = slice(cc * 512, (cc + 1) * 512)
                    Kc = T["Kown"](cc) if j is None else T["Kpast"](j, cc)
                    Vc = T["Vown"](cc) if j is None else T["Vpast"](j, cc)
                    kn, knb = knr.next(); kr, krb = krr.next(); vv, vvb = vvr.next()
                    c.dma("sp", kn[:], Kc[256 * hp:256 * hp + 256, :].rearrange("(h d) t -> d h t", h=2), writes=[knb], sembuf=knb)
                    c.dma("sp", kr[:], Kc[512:576, :], writes=[krb], sembuf=krb)
                    c.dma("sp", vv[:], Vc[:, 256 * hp:256 * hp + 256].rearrange("(b p) d -> p b d", p=128), writes=[vvb], sembuf=vvb)
                    diag = (j is None and cc == i)
                    bias = ce[:, 2:3] if j is None else qb[:, j:j + 1]
                    for kb in range(4):
                        q0 = 128 * kb if diag else 0
                        for hh in range(2):
                            ps, psb = Sr.next()
                            c.op("pe", lambda e: e.matmul(ps[:, q0:TT], kn[:, hh, kb * 128:(kb + 1) * 128], qn[:, hh, q0:TT], start=True, stop=False),
                                 reads=[knb, qnb], writes=[psb])
                            c.op("pe", lambda e: e.matmul(ps[:, q0:TT], kr[:, kb * 128:(kb + 1) * 128], qr[:, hh, q0:TT], start=False, stop=True),
                                 reads=[krb, qrb], writes=[psb], pe_chain=True)
                            pT, pTb = ptr_.next()
                            c.op("act", lambda e: e.activation(out=pT[:, q0:TT], in_=ps[:, q0:TT], func=AF.Exp, bias=bias, scale=ATT_SCALE),
                                 reads=[psb, qbb, ceb], writes=[pTb])
                            if diag:
                                c.op("pool", lambda e: e.memset(pT[64:128, q0:q0 + 64], 0.0), reads=[], writes=[pTb])
                            (O, Ob), (L, Lb) = Or[hh], Lr[hh]
                            c.op("pe", lambda e: e.matmul(O[:, q0:TT], vv[:, kb, hh * 128:(hh + 1) * 128], pT[:, q0:TT], start=(bi == 0), stop=(bi == nblk - 1)),
                                 reads=[vvb, pTb], writes=[Ob], pe_chain=(bi > 0))
                            c.op("pe", lambda e: e.matmul(L[:, q0:TT], ones[:], pT[:, q0:TT], start=(bi == 0), stop=(bi == nblk - 1)),
                                 reads=[onesb, pTb], writes=[Lb], pe_chain=(bi > 0))
                        bi += 1
                for hh in range(2):
                    (O, Ob), (L, Lb) = Or[hh], Lr[hh]
                    c.op("dve", lambda e: e.reciprocal(out=rl[:], in_=L[:]), reads=[Lb], writes=[rlb])
                    c.op("dve", lambda e: e.tensor_tensor(out=ya[:, 2 * hp + hh, :], in0=O[:], in1=rl[:], op=ALU.mult), reads=[Ob, rlb], writes=[yab])
            for h in range(4):
                c.op("act", lambda e: e.activation(out=sq[:, h, :], in_=ya[:, h, :], func=AF.Square), reads=[yab], writes=[sqb])
            pss, pssb = Sr.next()
            for h in range(4):
                c.op("pe", lambda e: e.matmul(pss[:], ones[:], sq[:, h, :], start=(h == 0), stop=(h == 3)), reads=[sqb, onesb], writes=[pssb], pe_chain=(h > 0))
            rstd_from(c, "act", rsF[:], pss[:], 1.0 / 512, ce[:, 0:1], [pssb, ceb], rsFb)
            for h in range(4):
                ob, obb = obr.next()
                c.op("dve", lambda e: e.scalar_tensor_tensor(out=ob[:], in0=ya[:, h, :], scalar=gn[:, h:h + 1], in1=rsF[:], op0=ALU.mult, op1=ALU.mult),
                     reads=[yab, gnb, rsFb], writes=[obb])
                c.dma("sp", T["mT0"][128 * h:128 * h + 128, cs], ob[:], reads=[obb], writes=[dm0], sembuf=obb)
        allb = [b for r in (qnr, qrr, knr, krr, vvr, ptr_, obr, Sr) for (_, b) in r.items] + [b for (_, b) in Or + Lr] + [gnb, qbb, yab, sqb, rlb, rsFb, K["pcol"][1]]
        c.barrier(allb)
        c.flush()
        c.stack = old
    return [dm0]


def layer_norm_rows(c, z, zb, bn, bnb, sts, stsb, gt, gtb, bt, btb, ce, ceb, out, outb):
    for k in range(4):
        c.op("dve", lambda e: e.bn_stats(out=bn[:, k, :], in_=z[:, 512 * k:512 * k + 512]), reads=[zb], writes=[bnb])
    c.op("dve", lambda e: e.bn_aggr(out=sts[:, 0:2], in_=bn[:].rearrange("p a b -> p (a b)")), reads=[bnb], writes=[stsb])
    rstd_from(c, "act", sts[:, 2:3], sts[:, 1:2], 1.0, ce[:, 1:2], [stsb, ceb], stsb)
    c.op("dve", lambda e: e.tensor_scalar(out=z[:], in0=z[:], scalar1=sts[:, 0:1], scalar2=sts[:, 2:3], op0=ALU.subtract, op1=ALU.mult),
         reads=[zb, stsb], writes=[zb])
    c.op("pool", lambda e: e.tensor_tensor(out=z[:], in0=z[:], in1=gt[:], op=ALU.mult), reads=[zb, gtb], writes=[zb])
    c.op("dve", lambda e: e.tensor_tensor(out=out[:], in0=z[:], in1=bt[:], op=ALU.add), reads=[zb, btb], writes=[outb])


def transpose_out(c, K, x1, x1b, pr, xTt, xTtb, x32=None):
    idf, idfb = K["idf"]
    for q in range(4):
        pt, ptb = pr.next()
        for k in range(4):
            cidx = 4 * q + k
            c.op("pe", lambda e: e.transpose(pt[:, 128 * k:128 * k + 128], x1[:, 128 * cidx:128 * cidx + 128], idf[:]), reads=[x1b, idfb], writes=[ptb], pe_chain=(k > 0))
        c.op("act", lambda e: e.activation(out=xTt[:, 4 * q:4 * q + 4, :], in_=pt[:].rearrange("p (k t) -> p k t", k=4), func=AF.Copy), reads=[ptb], writes=[xTtb])
        if x32 is not None:
            c.op("dve", lambda e: e.tensor_copy(out=x32[0][:, 4 * q:4 * q + 4, :], in_=pt[:].rearrange("p (k t) -> p k t", k=4)), reads=[ptb], writes=[x32[1]])


def phase_b1b(c, K, NT, T, moe):
    ce, ceb = K["ce"]
    dx1 = Buf("dram_x1"); dx1T = Buf("dram_x1T"); dlg = Buf("dram_lg")
    with contextlib.ExitStack() as st:
        c.stack, old = st, c.stack
        wo, wob = c.sbuf("wo", [128, 16, 2048], BF16)
        wov = T["w_o"].rearrange("(c p) n -> p c n", p=128)
        for k0 in range(0, 16, 2):
            c.dma("pool", wo[:, k0:k0 + 2, :], wov[:, k0:k0 + 2, :], writes=[wob], sembuf=wob)
        g1, g1b = c.sbuf("g1", [128, 2048], F32)
        b1, b1b = c.sbuf("b1", [128, 2048], F32)
        c.dma("sp", g1[:], T["ln_g"].partition_broadcast(128), writes=[g1b], sembuf=g1b)
        c.dma("sp", b1[:], T["ln_b"].partition_broadcast(128), writes=[b1b], sembuf=b1b)
        if moe:
            rw, rwb = c.sbuf("rw", [128, 16, 8], F32)
            c.dma("sp", rw[:], T["router_w"].rearrange("(c p) e -> p c e", p=128), writes=[rwb], sembuf=rwb)
            x32 = c.sbuf("x32", [128, 16, 128], F32)
            lgt, lgtb = c.sbuf("lgt", [128, 8], F32)
        mtr = Ring(c, "mt", [128, 16, TT], BF16, 2)
        xsr = Ring(c, "xs", [128, 2048], F32, 2)
        z, zb = c.sbuf("z", [128, 2048], F32)
        x1r = Ring(c, "x1", [128, 2048], F32, 2)
        xTr = Ring(c, "xTt", [128, 16, 128], BF16, 2)
        bn, bnb = c.sbuf("bn", [128, 4, 6], F32)
        sts, stsb = c.sbuf("sts", [128, 8], F32)
        pr = Ring(c, "pA", [128, 512], F32, 6, psum=True)
        plg, plgb = c.psum("plg", [128, 8], F32)
        for i in range(NT):
            cs = slice(i * TT, (i + 1) * TT)
            mt, mtb = mtr.next()
            c.dma("sp", mt[:, 0:4, :], T["mT0"][:, cs].rearrange("(c p) t -> p c t", p=128), writes=[mtb], sembuf=mtb)
            c.dma("sp", mt[:, 4:16, :], T["mT"][:, cs].rearrange("(c p) t -> p c t", p=128), writes=[mtb], sembuf=mtb)
            for sub in range(4):
                t0 = i * TT + sub * 128
                xs, xsb = xsr.next()
                c.dma("sp", xs[:], T["x"][t0:t0 + 128, :], writes=[xsb], sembuf=xsb)
                for n in range(4):
                    pp, ppb = pr.next()
                    for k in range(16):
                        c.op("pe", lambda e: e.matmul(pp[:], mt[:, k, sub * 128:(sub + 1) * 128], wo[:, k, 512 * n:512 * n + 512], start=(k == 0), stop=(k == 15)),
                             reads=[mtb, wob], writes=[ppb], pe_chain=(k > 0))
                    c.op("dve", lambda e: e.scalar_tensor_tensor(out=z[:, 512 * n:512 * n + 512], in0=xs[:, 512 * n:512 * n + 512], scalar=ALPHA, in1=pp[:], op0=ALU.mult, op1=ALU.add),
                         reads=[xsb, ppb], writes=[zb])
                x1, x1b = x1r.next()
                layer_norm_rows(c, z, zb, bn, bnb, sts, stsb, g1, g1b, b1, b1b, ce, ceb, x1, x1b)
                c.dma("sp", T["x1"][t0:t0 + 128, :], x1[:], reads=[x1b], writes=[dx1], sembuf=x1b)
                xTt, xTtb = xTr.next()
                transpose_out(c, K, x1, x1b, pr, xTt, xTtb, x32 if moe else None)
                c.dma("sp", T["x1T"][:, t0:t0 + 128].rearrange("(c p) t -> p c t", p=128), xTt[:], reads=[xTtb], writes=[dx1T], sembuf=xTtb)
                if moe:
                    for k in range(16):
                        c.op("pe", lambda e: e.matmul(plg[:], x32[0][:, k, :], rw[:, k, :], start=(k == 0), stop=(k == 15)), reads=[x32[1], rwb], writes=[plgb], pe_chain=(k > 0))
                    c.op("dve", lambda e: e.tensor_copy(out=lgt[:], in_=plg[:]), reads=[plgb], writes=[lgtb])
                    c.dma("sp", T["logits"][t0:t0 + 128, :], lgt[:], reads=[lgtb], writes=[dlg], sembuf=lgtb)
        allb = [b for r in (mtr, xsr, x1r, xTr, pr) for (_, b) in r.items] + [wob, g1b, b1b, zb, bnb, stsb, plgb]
        if moe:
            allb += [rwb, x32[1], lgtb]
        c.barrier(allb)
        c.flush()
        c.stack = old
    return [dx1, dx1T, dlg]


def phase_b2(c, K, NT, T, moe):
    ce, ceb = K["ce"]
    idf, idfb = K["idf"]
    dxo = Buf("dram_xo"); dxoT = Buf("dram_xoT")
    NE = 8 if moe else 1
    with contextlib.ExitStack() as st:
        c.stack, old = st, c.stack
        g2, g2b = c.sbuf("g2", [128, 2048], F32)
        b2, b2b = c.sbuf("b2", [128, 2048], F32)
        c.dma("sp", g2[:], T["ln_g"].partition_broadcast(128), writes=[g2b], sembuf=g2b)
        c.dma("sp", b2[:], T["ln_b"].partition_broadcast(128), writes=[b2b], sembuf=b2b)
        xt, xtb = c.sbuf("xt", [128, 16, TT], BF16)
        hT, hTb = c.sbuf("hT", [128, NF, TT], BF16)
        sg, sgb = c.sbuf("sg", [128, TT], F32)
        z, zb = c.sbuf("z", [128, 4, 2048], F32)
        x1r = Ring(c, "xo", [128, 2048], F32, 1)
        xTr = Ring(c, "xTt", [128, 16, 128], BF16, 1)
        bn, bnb = c.sbuf("bn", [128, 4, 6], F32)
        sts, stsb = c.sbuf("sts", [128, 16], F32)
        if moe:
            lg, lgb = c.sbuf("lg", [128, 8], F32)
            mx, mxb = c.sbuf("mx", [128, 8], F32)
            gt, gtb = c.sbuf("gt", [128, 8], F32)
            gT, gTb = c.sbuf("gT", [8, TT], F32)
            sel, selb = c.sbuf("sel", [8, 8, 128], F32)
            gbc, gbcb = c.sbuf("gbc", [128, 8, TT], BF16)
            c.op("pool", lambda e: e.memset(sel[:], 1.0), writes=[selb])
            c.op("pool", lambda e: e.affine_select(out=sel[:], in_=sel[:], pattern=[[-1, 8], [0, 128]], compare_op=ALU.is_equal, fill=0.0, base=0, channel_multiplier=1),
                 reads=[selb], writes=[selb])
        pr = Ring(c, "pA", [128, 512], F32, 8, psum=True)
        dws = Buf("dram_wscratch")
        with contextlib.ExitStack() as st3:
            c.stack, old3 = st3, c.stack
            wgr = Ring(c, "wgf", [128, 16, 128], F32, 2)
            wur = Ring(c, "wuf", [128, 16, 128], F32, 2)
            wgbr0 = Ring(c, "wgb0", [128, 16, 128], BF16, 2)
            wubr0 = Ring(c, "wub0", [128, 16, 128], BF16, 2)
            wdr = Ring(c, "wdf", [128, 2048], F32, 1)
            wdbr0 = Ring(c, "wdb0", [128, 2048], BF16, 2)
            for ex in range(NE):
                wgv = (T["wg"][ex] if moe else T["wg"]).rearrange("(c p) f -> p c f", p=128)
                wuv = (T["wu"][ex] if moe else T["wu"]).rearrange("(c p) f -> p c f", p=128)
                wdv = (T["wd"][ex] if moe else T["wd"])
                for f in range(NF):
                    fs = slice(f * 128, (f + 1) * 128)
                    wgf, wgfb = wgr.next(); wuf, wufb = wur.next(); wdf, wdfb = wdr.next()
                    c.dma("sp", wgf[:], wgv[:, :, fs], writes=[wgfb], sembuf=wgfb)
                    c.dma("sp", wuf[:], wuv[:, :, fs], writes=[wufb], sembuf=wufb)
                    c.dma("sp", wdf[:], wdv[f * 128:(f + 1) * 128, :], writes=[wdfb], sembuf=wdfb)
                    wgb0, wgb0b = wgbr0.next(); wub0, wub0b = wubr0.next(); wdb0, wdb0b = wdbr0.next()
                    c.op("act", lambda e: e.activation(out=wgb0[:], in_=wgf[:], func=AF.Copy), reads=[wgfb], writes=[wgb0b])
                    c.op("pool", lambda e: e.tensor_copy(out=wub0[:], in_=wuf[:]), reads=[wufb], writes=[wub0b])
                    c.op("dve", lambda e: e.tensor_copy(out=wdb0[:], in_=wdf[:]), reads=[wdfb], writes=[wdb0b])
                    widx = ex * NF + f
                    c.dma("sp", T["wgs"][widx].rearrange("p (k c) -> p k c", k=16), wgb0[:], reads=[wgb0b], writes=[dws], sembuf=wgb0b)
                    c.dma("sp", T["wus"][widx].rearrange("p (k c) -> p k c", k=16), wub0[:], reads=[wub0b], writes=[dws], sembuf=wub0b)
                    c.dma("sp", T["wds"][widx], wdb0[:], reads=[wdb0b], writes=[dws], sembuf=wdb0b)
            c.barrier([b for r in (wgr, wur, wgbr0, wubr0, wdr, wdbr0) for (_, b) in r.items])
            c.flush()
            c.stack = old3
        wgbr = Ring(c, "wgb", [128, 16, 128], BF16, 3)
        wubr = Ring(c, "wub", [128, 16, 128], BF16, 3)
        wdbr = Ring(c, "wdb", [128, 1024], BF16, 3)
        for i in range(NT):
            cs = slice(i * TT, (i + 1) * TT)
            c.dma("sp", xt[:], T["x1T"][:, cs].rearrange("(c p) t -> p c t", p=128), writes=[xtb], sembuf=xtb)
            for sub in range(4):
                t0 = i * TT + sub * 128
                c.dma("sp", z[:, sub, :], T["x1"][t0:t0 + 128, :], writes=[zb], sembuf=zb)
            for sub in range(4):
                c.op("act", lambda e: e.activation(out=z[:, sub, :], in_=z[:, sub, :], func=AF.Copy, scale=ALPHA), reads=[zb], writes=[zb])
            if moe:
                for sub in range(4):
                    t0 = i * TT + sub * 128
                    c.dma("sp", lg[:], T["logits"][t0:t0 + 128, :], writes=[lgb], sembuf=lgb)
                    c.op("dve", lambda e: e.max(out=mx[:], in_=lg[:]), reads=[lgb], writes=[mxb])
                    c.op("dve", lambda e: e.tensor_scalar(out=gt[:], in0=lg[:], scalar1=mx[:, 1:2], scalar2=None, op0=ALU.is_ge), reads=[lgb, mxb], writes=[gtb])
                    c.op("dve", lambda e: e.tensor_scalar(out=sts[:, 8:9], in0=mx[:, 0:1], scalar1=-1.0, scalar2=None, op0=ALU.mult), reads=[mxb], writes=[stsb])
                    c.op("act", lambda e: e.activation(out=lg[:], in_=lg[:], func=AF.Exp, bias=sts[:, 8:9], scale=1.0), reads=[lgb, stsb], writes=[lgb])
                    c.op("dve", lambda e: e.tensor_tensor(out=gt[:], in0=gt[:], in1=lg[:], op=ALU.mult), reads=[gtb, lgb], writes=[gtb])
                    c.op("dve", lambda e: e.tensor_reduce(out=sts[:, 9:10], in_=gt[:], axis=AX.X, op=ALU.add), reads=[gtb], writes=[stsb])
                    c.op("dve", lambda e: e.reciprocal(out=sts[:, 10:11], in_=sts[:, 9:10]), reads=[stsb], writes=[stsb])
                    c.op("dve", lambda e: e.tensor_scalar(out=gt[:], in0=gt[:], scalar1=sts[:, 10:11], scalar2=None, op0=ALU.mult), reads=[gtb, stsb], writes=[gtb])
                    pp, ppb = pr.next()
                    c.op("pe", lambda e: e.transpose(pp[0:8, 0:128], gt[:], idf[:]), reads=[gtb, idfb], writes=[ppb])
                    c.op("act", lambda e: e.activation(out=gT[:, sub * 128:(sub + 1) * 128], in_=pp[0:8, 0:128], func=AF.Copy), reads=[ppb], writes=[gTb])
                for ex in range(8):
                    pp, ppb = pr.next()
                    c.op("pe", lambda e: e.matmul(pp[:], sel[:, ex, :], gT[:], start=True, stop=True), reads=[selb, gTb], writes=[ppb])
                    c.op("act", lambda e: e.activation(out=gbc[:, ex, :], in_=pp[:], func=AF.Copy), reads=[ppb], writes=[gbcb])
            for ex in range(NE):
                wgv = (T["wg"][ex] if moe else T["wg"]).rearrange("(c p) f -> p c f", p=128)
                wuv = (T["wu"][ex] if moe else T["wu"]).rearrange("(c p) f -> p c f", p=128)
                wdv = (T["wd"][ex] if moe else T["wd"])
                for f in range(NF):
                    fs = slice(f * 128, (f + 1) * 128)
                    wgb_, wgbb = wgbr.next(); wub_, wubb = wubr.next()
                    c.dma("sp", wgb_[:], T["wgs"][ex * NF + f].rearrange("p (k c) -> p k c", k=16), writes=[wgbb], sembuf=wgbb)
                    c.dma("sp", wub_[:], T["wus"][ex * NF + f].rearrange("p (k c) -> p k c", k=16), writes=[wubb], sembuf=wubb)
                    pg, pgb = pr.next(); pu, pub = pr.next()
                    for k in range(16):
                        c.op("pe", lambda e: e.matmul(pg[:], wgb_[:, k, :], xt[:, k, :], start=(k == 0), stop=(k == 15)), reads=[wgbb, xtb], writes=[pgb], pe_chain=(k > 0))
                    for k in range(16):
                        c.op("pe", lambda e: e.matmul(pu[:], wub_[:, k, :], xt[:, k, :], start=(k == 0), stop=(k == 15)), reads=[wubb, xtb], writes=[pub], pe_chain=(k > 0))
                    c.op("act", lambda e: e.activation(out=sg[:], in_=pg[:], func=AF.Silu), reads=[pgb], writes=[sgb])
                    if moe:
                        c.op("dve", lambda e: e.tensor_tensor(out=sg[:], in0=sg[:], in1=gbc[:, ex, :], op=ALU.mult), reads=[sgb, gbcb], writes=[sgb])
                    c.op("dve", lambda e: e.tensor_tensor(out=hT[:, f, :], in0=pu[:], in1=sg[:], op=ALU.mult), reads=[pub, sgb], writes=[hTb])
                for nh in range(2):
                    accs = [pr.next() for _ in range(8)]
                    for f in range(NF):
                        wdb_, wdbb = wdbr.next()
                        c.dma("sp", wdb_[:], T["wds"][ex * NF + f][:, nh * 1024:(nh + 1) * 1024], writes=[wdbb], sembuf=wdbb)
                        for sub in range(4):
                            for n2 in range(2):
                                pa_, pab = accs[sub * 2 + n2]
                                c.op("pe", lambda e: e.matmul(pa_[:], hT[:, f, sub * 128:(sub + 1) * 128], wdb_[:, n2 * 512:(n2 + 1) * 512], start=(f == 0), stop=(f == NF - 1)),
                                     reads=[hTb, wdbb], writes=[pab], pe_chain=(f > 0))
                    for sub in range(4):
                        for n2 in range(2):
                            pa_, pab = accs[sub * 2 + n2]
                            col = nh * 1024 + n2 * 512
                            c.op("dve", lambda e: e.tensor_tensor(out=z[:, sub, col:col + 512], in0=z[:, sub, col:col + 512], in1=pa_[:], op=ALU.add), reads=[zb, pab], writes=[zb])
            for sub in range(4):
                t0 = i * TT + sub * 128
                xo, xob = x1r.next()
                zs = z[:, sub, :]
                layer_norm_rows(c, zs, zb, bn, bnb, sts, stsb, g2, g2b, b2, b2b, ce, ceb, xo, xob)
                c.dma("sp", T["xo"][t0:t0 + 128, :], xo[:], reads=[xob], writes=[dxo], sembuf=xob)
                xTt, xTtb = xTr.next()
                transpose_out(c, K, xo, xob, pr, xTt, xTtb)
                c.dma("sp", T["xoT"][:, t0:t0 + 128].rearrange("(c p) t -> p c t", p=128), xTt[:], reads=[xTtb], writes=[dxoT], sembuf=xTtb)
        allb = [b for r in (wgbr, wubr, wdbr, x1r, xTr, pr) for (_, b) in r.items] + [g2b, b2b, xtb, hTb, sgb, zb, bnb, stsb]
        if moe:
            allb += [lgb, mxb, gtb, gTb, selb, gbcb]
        c.barrier(allb)
        c.flush()
        c.stack = old
    return [dxo, dxoT]


import ml_dtypes
NCORE = 8
GROUPS = [[0, 1, 2, 3], [4, 5, 6, 7]]


def _di(nc, name, shape, dt):
    return nc.dram_tensor(name, list(shape), dt, kind="ExternalInput").ap()


def _do(nc, name, shape, dt):
    return nc.dram_tensor(name, list(shape), dt, kind="ExternalOutput").ap()


def _dint(nc, name, shape, dt):
    return nc.dram_tensor(name, list(shape), dt).ap()


def build_fused(NT):
    NTOK = NT * TT
    nc = bass.Bass("TRN2", target_bir_lowering=False, num_devices=NCORE)
    L = 4
    I = dict(
        x=_di(nc, "x", [NTOK, 2048], F32), pos=_di(nc, "pos", [NTOK], I32), invf=_di(nc, "invf", [64], F32),
        invc=_di(nc, "invc", [4, XH], F32), qbias=_di(nc, "qbias", [128, 3], F32), hsel=_di(nc, "hsel", [128, 4], F32),
        w_in=_di(nc, "w_in", [L, 2048, 4416], F32), kvg=_di(nc, "kv_norm_g", [L, 512], F32), w_ukv=_di(nc, "w_ukv", [L, 512, 1024], F32),
        conv_w=_di(nc, "conv_w", [L, 3, 512], F32), pool_w=_di(nc, "pool_w", [L, 4, 128, 128], F32),
        sgu_ln_g=_di(nc, "sgu_ln_g", [L, 512], F32), sgu_ln_b=_di(nc, "sgu_ln_b", [L, 512], F32),
        sgu_w=_di(nc, "sgu_w", [L, 4, 128, 128], F32), sgu_b=_di(nc, "sgu_b", [L, 4, 128], F32), gain=_di(nc, "mix_gain", [L, 2048], F32),
        w_o=_di(nc, "w_o", [L, 2048, 2048], F32), ln1g=_di(nc, "ln1_g", [L, 2048], F32), ln1b=_di(nc, "ln1_b", [L, 2048], F32),
        ln2g=_di(nc, "ln2_g", [L, 2048], F32), ln2b=_di(nc, "ln2_b", [L, 2048], F32),
        ffn_wg=_di(nc, "ffn_wg", [2, 2048, DFF], F32), ffn_wu=_di(nc, "ffn_wu", [2, 2048, DFF], F32), ffn_wd=_di(nc, "ffn_wd", [2, DFF, 2048], F32),
        router_w=_di(nc, "router_w", [2, 2048, 8], F32),
        exp_wg=_di(nc, "exp_wg", [2, 8, 2048, DFF], F32), exp_wu=_di(nc, "exp_wu", [2, 8, 2048, DFF], F32), exp_wd=_di(nc, "exp_wd", [2, 8, DFF, 2048], F32),
    )
    out = _do(nc, "out", [NTOK, 2048], F32)
    xT = _dint(nc, "xT", [2048, XH + NTOK], BF16)
    xbuf = _dint(nc, "xbuf", [NTOK, 2048], F32)
    QT = _dint(nc, "QT", [4, 192, NTOK], BF16); KT = _dint(nc, "KT", [NT, 576, TT], BF16); V = _dint(nc, "V", [NTOK, 512], BF16)
    mT = _dint(nc, "mT", [1536, NTOK], BF16); mT0 = _dint(nc, "mT0", [512, NTOK], BF16)
    KTall = _dint(nc, "KTall", [NT, 4 * 576, TT], BF16); Vall = _dint(nc, "Vall", [NT, 4 * TT, 512], BF16)
    x1 = _dint(nc, "x1", [NTOK, 2048], F32); x1T = _dint(nc, "x1T", [2048, NTOK], BF16); logits = _dint(nc, "logits", [NTOK, 8], F32)
    hsrc = _dint(nc, "hsrc", [2048, XH], BF16); hall = _dint(nc, "hall", [4 * 2048, XH], BF16)
    wgs = _dint(nc, "wgs", [8 * NF, 128, 2048], BF16); wus = _dint(nc, "wus", [8 * NF, 128, 2048], BF16)
    wds = _dint(nc, "wds", [8 * NF, 128, 2048], BF16)
    with contextlib.ExitStack() as st:
        c = Ctx(nc, st)
        K = consts(c)
        d = Buf("dram_misc")
        with contextlib.ExitStack() as st2:
            c.stack, old = st2, c.stack
            xr = Ring(c, "xs", [128, 2048], F32, 2)
            xTr = Ring(c, "xTt", [128, 16, 128], BF16, 2)
            pr = Ring(c, "pA", [128, 512], F32, 4, psum=True)
            for t in range(NTOK // 128):
                xs, xsb = xr.next()
                c.dma("sp", xs[:], I["x"][t * 128:(t + 1) * 128, :], writes=[xsb], sembuf=xsb)
                xTt, xTtb = xTr.next()
                transpose_out(c, K, xs, xsb, pr, xTt, xTtb)
                c.dma("sp", xT[:, XH + t * 128:XH + (t + 1) * 128].rearrange("(c p) t -> p c t", p=128), xTt[:], reads=[xTtb], writes=[d], sembuf=xTtb)
            c.barrier([b for r in (xr, xTr, pr) for (_, b) in r.items])
            c.flush()
            c.stack = old
        for l in range(L):
            moe = (l % 2 == 1)
            with contextlib.ExitStack() as st2:
                c.stack, old = st2, c.stack
                hs0, hs0b = c.sbuf("hs0", [128, 16, XH], BF16)
                hl, hlb = c.sbuf("hl", [128, 4, 16, XH], BF16)
                hacc, haccb = c.sbuf("hacc", [128, 16, XH], F32)
                hsl, hslb = c.sbuf("hsel", [128, 4], F32)
                dh = Buf("dram_h"); dha = Buf("dram_ha")
                c.dma("sp", hsl[:], I["hsel"], writes=[hslb], sembuf=hslb)
                c.dma("sp", hs0[:], xT[:, NTOK:NTOK + XH].rearrange("(c p) t -> p c t", p=128), writes=[hs0b], sembuf=hs0b)
                c.dma("sp", hsrc.rearrange("(c p) t -> p c t", p=128), hs0[:], reads=[hs0b], writes=[dh], sembuf=hs0b)
                c.barrier([])
                c.collective("AllGather", hsrc[:, :], hall[:, :], GROUPS, reads=[dh], writes=[dha], sembuf=dha)
                c.barrier([])
                hv = hall.rearrange("(j c p) t -> p j c t", j=4, p=128)
                for j in range(4):
                    c.dma("sp", hl[:, j, :, :], hv[:, j, :, :], reads=[dha], writes=[hlb], sembuf=hlb)
                c.op("dve", lambda e: e.tensor_scalar(out=hacc[:], in0=hl[:, 0, :, :], scalar1=hsl[:, 0:1], scalar2=None, op0=ALU.mult), reads=[hlb, hslb], writes=[haccb])
                for j in range(1, 4):
                    c.op("dve", lambda e: e.scalar_tensor_tensor(out=hacc[:], in0=hl[:, j, :, :], scalar=hsl[:, j:j + 1], in1=hacc[:], op0=ALU.mult, op1=ALU.add),
                         reads=[hlb, hslb, haccb], writes=[haccb])
                c.op("dve", lambda e: e.tensor_copy(out=hs0[:], in_=hacc[:]), reads=[haccb], writes=[hs0b])
                c.dma("sp", xT[:, 0:XH].rearrange("(c p) t -> p c t", p=128), hs0[:], reads=[hs0b], writes=[d], sembuf=hs0b)
                c.barrier([hs0b, hlb, haccb, hslb, dha, dh])
                c.flush()
                c.stack = old
            TA = dict(xT=xT, pos=I["pos"], invf=I["invf"], w_in=I["w_in"][l], kvg=I["kvg"][l], w_ukv=I["w_ukv"][l], conv_w=I["conv_w"][l],
                      pool_w=I["pool_w"][l], sgu_ln_g=I["sgu_ln_g"][l], sgu_ln_b=I["sgu_ln_b"][l], sgu_w=I["sgu_w"][l], sgu_b=I["sgu_b"][l],
                      gain=I["gain"][l], invc=I["invc"], QT=QT, KTt=(lambda i: KT[i]), V=V, mT=mT)
            phase_a(c, K, NT, TA)
            dka = Buf("dram_ka"); dva = Buf("dram_va")
            for ch in range(NT):
                c.collective("AllGather", KT[ch], KTall[ch], GROUPS, writes=[dka], sembuf=dka)
                c.collective("AllGather", V[ch * TT:(ch + 1) * TT, :], Vall[ch], GROUPS, writes=[dva], sembuf=dva)
            c.barrier([dka, dva])
            c.flush()
            TB = dict(QT=QT, Kown=(lambda cc: KT[cc]), Vown=(lambda cc: V[cc * TT:(cc + 1) * TT, :]),
                      Kpast=(lambda j, cc: KTall[cc].rearrange("(j r) t -> j r t", j=4)[j]),
                      Vpast=(lambda j, cc: Vall[cc].rearrange("(j s) d -> j s d", j=4)[j]),
                      qbias=I["qbias"], gain=I["gain"][l], mT=mT, x=(I["x"] if l == 0 else xbuf), w_o=I["w_o"][l],
                      mT0=mT0, x1=x1, x1T=x1T, logits=logits, xo=(out if l == L - 1 else xbuf), xoT=xT[:, XH:XH + NTOK])
            phase_b1a(c, K, NT, 3, TB)
            phase_b1b(c, K, NT, dict(TB, ln_g=I["ln1g"][l], ln_b=I["ln1b"][l], router_w=(I["router_w"][l // 2] if moe else None)), moe)
            if moe:
                TW = dict(wg=I["exp_wg"][l // 2], wu=I["exp_wu"][l // 2], wd=I["exp_wd"][l // 2])
            else:
                TW = dict(wg=I["ffn_wg"][l // 2], wu=I["ffn_wu"][l // 2], wd=I["ffn_wd"][l // 2])
            phase_b2(c, K, NT, dict(TB, ln_g=I["ln2g"][l], ln_b=I["ln2b"][l], wgs=wgs, wus=wus, wds=wds, **TW), moe)
        c.finish([])
    return nc


def kernel(x, positions, w_in, kv_norm_g, w_ukv, conv_w, pool_w, sgu_ln_g, sgu_ln_b, sgu_w, sgu_b,
           mix_gain, w_o, ln1_g, ln1_b, ffn_wg, ffn_wu, ffn_wd, router_w, exp_wg, exp_wu, exp_wd, ln2_g, ln2_b):
    A = lambda a: np.ascontiguousarray(np.asarray(a, np.float32))
    x = np.asarray(x, np.float32)
    positions = np.asarray(positions).astype(np.int32)
    S = x.shape[1]
    NTOK = S // 4
    NT = NTOK // TT
    cores = [(c // 4, c % 4) for c in range(NCORE)]
    invf = (10000.0 ** (-np.arange(0, 64, 2, dtype=np.float32) / 64)).astype(np.float32)
    invf = np.concatenate([invf, invf]).astype(np.float32)
    shared = dict(invf=invf, w_in=A(w_in), kv_norm_g=A(kv_norm_g), w_ukv=A(w_ukv), conv_w=A(conv_w), pool_w=A(pool_w), sgu_ln_g=A(sgu_ln_g),
                  sgu_ln_b=A(sgu_ln_b), sgu_w=A(sgu_w), sgu_b=A(sgu_b), mix_gain=A(mix_gain), w_o=A(w_o), ln1_g=A(ln1_g), ln1_b=A(ln1_b),
                  ln2_g=A(ln2_g), ln2_b=A(ln2_b), ffn_wg=A(ffn_wg), ffn_wu=A(ffn_wu), ffn_wd=A(ffn_wd), router_w=A(router_w),
                  exp_wg=A(exp_wg), exp_wu=A(exp_wu), exp_wd=A(exp_wd))
    in_maps = []
    for (b, r) in cores:
        t_abs = r * NTOK + np.arange(XH)
        invc = np.stack([1.0 / np.minimum(t_abs + 1, w) for w in (2, 4, 8, 16)]).astype(np.float32)
        qbias = np.broadcast_to(np.where(np.arange(3) < r, 0.0, -30000.0).astype(np.float32)[None, :], (128, 3)).copy()
        hsel = np.zeros((128, 4), np.float32)
        if r > 0:
            hsel[:, r - 1] = 1.0
        in_maps.append(dict(shared, x=np.ascontiguousarray(x[b, r * NTOK:(r + 1) * NTOK]),
                            pos=np.ascontiguousarray(positions[b, r * NTOK:(r + 1) * NTOK]), invc=invc, qbias=qbias, hsel=hsel))
    nc = build_fused(NT)
    res = run_bass_kernel_spmd(nc, in_maps, core_ids=list(range(NCORE)))
    out = np.empty((2, S, 2048), np.float32)
    for c, (b, r) in enumerate(cores):
        out[b, r * NTOK:(r + 1) * NTOK] = np.asarray(res.results[c]["out"])
    return out
```
